# Optimizing a Trainium2 kernel written in Bass

```python
import jax, jax.numpy as jnp
from jax import lax
import numpy as np

D_MODEL = 1024
BATCH = 16
SEQ = 4096
DEPTH = 2
DEC_BATCH = 2
DEC_SEQ = 8192
PAST_LEN = 128

EPS = 1e-6
MLA_HEADS = 4
MLA_Q_LORA = 256
MLA_KV_LORA = 128
MLA_NOPE = 64
MLA_ROPE = 32
MLA_V = 64
MLA_THETA = 10000.0
ATTN_QBLK = 128
FNET_GROUPS = 4
FNET_CH = 64
GLA_HEADS = 4
GLA_DK = 32
GLA_DV = 64
GLA_GATE_RANK = 16
GLA_TAU = 16.0
GLA_CHUNK = 64
DIL_HEADS = 4
DIL_DH = 64
DIL_PATTERNS = ((128, 1), (512, 4), (2048, 16))
ROPE_THETA = 500000.0
ROPE_DIMS = DIL_DH // 4
N_EXPERTS = 32
TOP_K = 4
D_EXPERT = D_MODEL
SWIGLU_LIMIT = 7.0
SWIGLU_ALPHA = 1.702
MOE_BLOCK = 512

IN_SPLITS = (MLA_Q_LORA, MLA_KV_LORA, MLA_ROPE,
             FNET_GROUPS * FNET_CH,
             GLA_HEADS * GLA_DK, GLA_HEADS * GLA_DK, GLA_HEADS * GLA_DV, GLA_HEADS * GLA_DV,
             GLA_GATE_RANK, GLA_GATE_RANK,
             DIL_HEADS * DIL_DH, DIL_HEADS * DIL_DH, DIL_HEADS * DIL_DH)
D_IN = sum(IN_SPLITS)
MIX_OUT = MLA_HEADS * MLA_V + FNET_GROUPS * FNET_CH + GLA_HEADS * GLA_DV + DIL_HEADS * DIL_DH

kernel_name = 'hybrid_parallel_mixer_encoder'


def rms_norm(x, g):
    xf = x.astype(jnp.float32)
    y = xf * lax.rsqrt(jnp.mean(xf * xf, axis=-1, keepdims=True) + EPS)
    return (y * g.astype(jnp.float32)).astype(x.dtype)


def rotary(x, theta):
    S, R = x.shape[1], x.shape[-1]
    inv = jnp.power(jnp.float32(theta), -jnp.arange(0, R, 2, dtype=jnp.float32) / R)
    ang = jnp.arange(S, dtype=jnp.float32)[:, None] * inv[None, :]
    bshape = (1, S) + (1,) * (x.ndim - 3) + (R // 2,)
    cos, sin = jnp.cos(ang).reshape(bshape), jnp.sin(ang).reshape(bshape)
    x1, x2 = jnp.split(x.astype(jnp.float32), 2, axis=-1)
    return jnp.concatenate([x1 * cos - x2 * sin, x1 * sin + x2 * cos], axis=-1).astype(x.dtype)


def dense_attention(q, k, v, scale):
    B, S, H, d = q.shape
    nq = S // ATTN_QBLK
    qb = q.reshape(B, nq, ATTN_QBLK, H, d).transpose(1, 0, 2, 3, 4)

    def one_block(qblk):
        s = jnp.einsum('bqhd,bkhd->bhqk', qblk, k).astype(jnp.float32) * scale
        p = jax.nn.softmax(s, axis=-1)
        return jnp.einsum('bhqk,bkhe->bqhe', p.astype(v.dtype), v)

    o = lax.map(one_block, qb)
    return o.transpose(1, 0, 2, 3, 4).reshape(B, S, H, v.shape[-1])


def mla_mixer(q_lat, kv_lat, k_rope, g_q, g_kv, w_uq, w_ukv):
    B, S, _ = q_lat.shape
    q = (rms_norm(q_lat, g_q) @ w_uq).reshape(B, S, MLA_HEADS, MLA_NOPE + MLA_ROPE)
    kv = (rms_norm(kv_lat, g_kv) @ w_ukv).reshape(B, S, MLA_HEADS, MLA_NOPE + MLA_V)
    q_nope, q_pe = q[..., :MLA_NOPE], rotary(q[..., MLA_NOPE:], MLA_THETA)
    k_nope, v = kv[..., :MLA_NOPE], kv[..., MLA_NOPE:]
    k_pe = rotary(k_rope, MLA_THETA)[:, :, None, :]
    qf = jnp.concatenate([q_nope, q_pe], axis=-1)
    kf = jnp.concatenate([k_nope, jnp.broadcast_to(k_pe, (B, S, MLA_HEADS, MLA_ROPE))], axis=-1)
    o = dense_attention(qf, kf, v, (MLA_NOPE + MLA_ROPE) ** -0.5)
    return o.reshape(B, S, MLA_HEADS * MLA_V)


def fourier_mixer(u):
    B, S, _ = u.shape
    uf = u.astype(jnp.float32).reshape(B, S, FNET_GROUPS, FNET_CH)
    y = jnp.fft.fft2(uf, axes=(1, 3), norm='ortho').real
    return y.reshape(B, S, FNET_GROUPS * FNET_CH).astype(u.dtype)


def gla_chunked(q, k, v, log_a):
    B, S, H, dk = q.shape
    dv = v.shape[-1]
    N = S // GLA_CHUNK
    ch = lambda t: t.reshape(B, N, GLA_CHUNK, H, t.shape[-1]).astype(jnp.float32)
    q, k, v, g = ch(q), ch(k), ch(v), ch(log_a)
    b = jnp.cumsum(g, axis=2)
    b_mid = b[:, :, GLA_CHUNK // 2:GLA_CHUNK // 2 + 1]
    b_last = b[:, :, -1:]
    qi = q * jnp.exp(b - b_mid)
    ki = k * jnp.exp(b_mid - b)
    causal = jnp.tril(jnp.ones((GLA_CHUNK, GLA_CHUNK), dtype=bool))
    a = jnp.einsum('bnihd,bnjhd->bnhij', qi, ki)
    a = jnp.where(causal, a, 0.0)
    o_intra = jnp.einsum('bnhij,bnjhe->bnihe', a, v)
    u = jnp.einsum('bnjhd,bnjhe->bnhde', k * jnp.exp(b_last - b), v)
    decay = jnp.exp(b_last[:, :, 0])

    def step(s, inp):
        u_n, d_n = inp
        return d_n[..., None] * s + u_n, s

    _, s_prev = lax.scan(step, jnp.zeros((B, H, dk, dv), jnp.float32),
                         (u.transpose(1, 0, 2, 3, 4), decay.transpose(1, 0, 2, 3)))
    s_prev = s_prev.transpose(1, 0, 2, 3, 4)
    o_inter = jnp.einsum('bnihd,bnhde->bnihe', q * jnp.exp(b), s_prev)
    return (o_intra + o_inter).reshape(B, S, H, dv)


def gla_mixer(q, k, v, r, zf, zb, w_gf, b_gf, w_gb, b_gb, g_out):
    B, S, _ = q.shape
    hd = lambda t, d: t.reshape(B, S, GLA_HEADS, d)
    q = hd(q, GLA_DK) * (GLA_DK ** -0.5)
    k, v = hd(k, GLA_DK), hd(v, GLA_DV)
    log_f = hd(jax.nn.log_sigmoid((zf @ w_gf + b_gf).astype(jnp.float32)) / GLA_TAU, GLA_DK)
    log_b = hd(jax.nn.log_sigmoid((zb @ w_gb + b_gb).astype(jnp.float32)) / GLA_TAU, GLA_DK)
    flip = lambda t: jnp.flip(t, axis=1)
    o = gla_chunked(q, k, v, log_f) + flip(gla_chunked(flip(q), flip(k), flip(v), flip(log_b)))
    o = rms_norm(o, g_out.reshape(GLA_HEADS, GLA_DV)).reshape(B, S, GLA_HEADS * GLA_DV)
    return (o * jax.nn.silu(r.astype(jnp.float32))).astype(r.dtype)


def banded_attention(q, k, v, half):
    N, L, H, dh = q.shape
    qb = half
    nb = -(-L // qb)
    lp = nb * qb
    qp = jnp.pad(q, ((0, 0), (0, lp - L), (0, 0), (0, 0))).reshape(N, nb, qb, H, dh)

    def windows(t):
        tb = jnp.pad(t, ((0, 0), (qb, lp - L + qb), (0, 0), (0, 0))).reshape(N, nb + 2, qb, H, t.shape[-1])
        return jnp.concatenate([tb[:, :-2], tb[:, 1:-1], tb[:, 2:]], axis=2)

    kw, vw = windows(k), windows(v)
    s = jnp.einsum('nbqhd,nbkhd->nbhqk', qp, kw).astype(jnp.float32) * (dh ** -0.5)
    blk = jnp.arange(nb)[:, None]
    qpos = blk * qb + jnp.arange(qb)[None, :]
    kpos = blk * qb - qb + jnp.arange(3 * qb)[None, :]
    kp3 = kpos[:, None, :]
    valid = (jnp.abs(kp3 - qpos[:, :, None]) <= half) & (kp3 >= 0) & (kp3 < L)
    s = jnp.where(valid[None, :, None], s, -jnp.inf)
    m = jnp.max(s, axis=-1, keepdims=True)
    p = jnp.exp(s - m)
    num = jnp.einsum('nbhqk,nbkhd->nbqhd', p, vw.astype(jnp.float32))
    den = p.sum(-1).transpose(0, 1, 3, 2)
    mx = m[..., 0].transpose(0, 1, 3, 2)
    trim = lambda t: t.reshape((N, lp) + t.shape[3:])[:, :L]
    return trim(num), trim(den), trim(mx)


def dilated_mixer(q, k, v):
    B, S, _ = q.shape
    hd = lambda t: t.reshape(B, S, DIL_HEADS, DIL_DH)
    q, k, v = hd(q), hd(k), hd(v)
    q = jnp.concatenate([rotary(q[..., :ROPE_DIMS], ROPE_THETA), q[..., ROPE_DIMS:]], axis=-1)
    k = jnp.concatenate([rotary(k[..., :ROPE_DIMS], ROPE_THETA), k[..., ROPE_DIMS:]], axis=-1)
    nums, dens, maxs = [], [], []
    for window, dil in DIL_PATTERNS:
        L = S // dil
        to_res = lambda t: t.reshape(B, L, dil, DIL_HEADS, t.shape[-1]).transpose(0, 2, 1, 3, 4).reshape(B * dil, L, DIL_HEADS, t.shape[-1])
        from_res = lambda t: t.reshape(B, dil, L, DIL_HEADS, t.shape[-1]).transpose(0, 2, 1, 3, 4).reshape(B, S, DIL_HEADS, t.shape[-1])
        num, den, mx = banded_attention(to_res(q), to_res(k), to_res(v), window // (2 * dil))
        nums.append(from_res(num))
        dens.append(from_res(den[..., None]))
        maxs.append(from_res(mx[..., None]))
    m_all = jnp.stack(maxs)
    w = jnp.exp(m_all - jnp.max(m_all, axis=0, keepdims=True))
    num = jnp.sum(w * jnp.stack(nums), axis=0)
    den = jnp.sum(w * jnp.stack(dens), axis=0)
    return (num / den).reshape(B, S, DIL_HEADS * DIL_DH).astype(v.dtype)


def clamped_swiglu(gu):
    g, lin = jnp.split(gu, 2, axis=-1)
    g = jnp.minimum(g, SWIGLU_LIMIT)
    lin = jnp.clip(lin, -SWIGLU_LIMIT, SWIGLU_LIMIT)
    return g * jax.nn.sigmoid(SWIGLU_ALPHA * g) * (lin + 1.0)


def moe_ffn(h, w_router, b_router, w_gu, b_gu, w_down, b_down):
    T, D = h.shape
    logits = (h @ w_router + b_router).astype(jnp.float32)
    top_logit, top_e = lax.top_k(logits, TOP_K)
    gate = jax.nn.softmax(top_logit, axis=-1)
    n_assign = T * TOP_K
    flat_e = top_e.reshape(-1)
    order = jnp.argsort(flat_e)
    e_sorted = flat_e[order]
    counts = jnp.bincount(flat_e, length=N_EXPERTS)
    padded = (counts + MOE_BLOCK - 1) // MOE_BLOCK * MOE_BLOCK
    pad_end = jnp.cumsum(padded)
    pad_start = pad_end - padded
    start = jnp.cumsum(counts) - counts
    dest = pad_start[e_sorted] + jnp.arange(n_assign) - start[e_sorted]
    n_blocks = -(-n_assign // MOE_BLOCK) + N_EXPERTS
    n_slots = n_blocks * MOE_BLOCK
    slot_tok = jnp.full((n_slots,), T, jnp.int32).at[dest].set((order // TOP_K).astype(jnp.int32))
    slot_gate = jnp.zeros((n_slots,), h.dtype).at[dest].set(gate.reshape(-1)[order].astype(h.dtype))
    blk_e = jnp.minimum(jnp.searchsorted(pad_end, jnp.arange(n_blocks) * MOE_BLOCK, side='right'), N_EXPERTS - 1)
    h_pad = jnp.concatenate([h, jnp.zeros((1, D), h.dtype)], axis=0)

    def expert_block(args):
        tok, e = args
        gu = h_pad[tok] @ w_gu[e] + b_gu[e]
        return clamped_swiglu(gu) @ w_down[e] + b_down[e]

    ys = lax.map(expert_block, (slot_tok.reshape(n_blocks, MOE_BLOCK), blk_e))
    out = jnp.zeros((T + 1, D), h.dtype).at[slot_tok].add(ys.reshape(n_slots, D) * slot_gate[:, None])
    return out[:T]


def encoder_layer(x, c, w_ada, b_ada, g_mix, g_ffn, w_in, mla_g_q, mla_g_kv, mla_w_uq, mla_w_ukv,
                  gla_w_gf, gla_b_gf, gla_w_gb, gla_b_gb, gla_g_out, w_out,
                  w_router, b_router, w_gu, b_gu, w_down, b_down):
    B, S, D = x.shape
    mod = (jax.nn.silu(c) @ w_ada + b_ada)[:, None, :]
    sh_m, sc_m, gt_m, sh_f, sc_f, gt_f = jnp.split(mod, 6, axis=-1)
    h = rms_norm(x, g_mix) * (1.0 + sc_m) + sh_m
    z = h @ w_in
    (q_lat, kv_lat, k_rope, u_fft, gq, gk, gv, gr, zf, zb, dq, dk, dv) = jnp.split(
        z, np.cumsum(IN_SPLITS)[:-1].tolist(), axis=-1)
    o = jnp.concatenate([
        mla_mixer(q_lat, kv_lat, k_rope, mla_g_q, mla_g_kv, mla_w_uq, mla_w_ukv),
        fourier_mixer(u_fft),
        gla_mixer(gq, gk, gv, gr, zf, zb, gla_w_gf, gla_b_gf, gla_w_gb, gla_b_gb, gla_g_out),
        dilated_mixer(dq, dk, dv),
    ], axis=-1)
    x = x + gt_m * (o @ w_out)
    h = rms_norm(x, g_ffn) * (1.0 + sc_f) + sh_f
    f = moe_ffn(h.reshape(B * S, D), w_router, b_router, w_gu, b_gu, w_down, b_down)
    return x + gt_f * f.reshape(B, S, D)


def setup_inputs(seed: int = 0) -> dict:
    key = jax.random.key(seed)
    ks = iter(jax.random.split(key, 32))
    nrm = lambda shape, scale: jax.random.normal(next(ks), shape, jnp.float32) * scale
    L, D, E, F = DEPTH, D_MODEL, N_EXPERTS, D_EXPERT
    return {
        'x_prompt': nrm((BATCH, SEQ, D), 1.0),
        'x_sample': nrm((DEC_BATCH, DEC_SEQ, D), 1.0),
        'c_prompt': nrm((BATCH, D), 1.0),
        'c_sample': nrm((DEC_BATCH, D), 1.0),
        'w_ada': nrm((L, D, 6 * D), 0.5 * D ** -0.5),
        'b_ada': nrm((L, 6 * D), 0.02),
        'g_mix': 1.0 + nrm((L, D), 0.05),
        'g_ffn': 1.0 + nrm((L, D), 0.05),
        'w_in': nrm((L, D, D_IN), D ** -0.5),
        'mla_g_q': 1.0 + nrm((L, MLA_Q_LORA), 0.05),
        'mla_g_kv': 1.0 + nrm((L, MLA_KV_LORA), 0.05),
        'mla_w_uq': nrm((L, MLA_Q_LORA, MLA_HEADS * (MLA_NOPE + MLA_ROPE)), MLA_Q_LORA ** -0.5),
        'mla_w_ukv': nrm((L, MLA_KV_LORA, MLA_HEADS * (MLA_NOPE + MLA_V)), MLA_KV_LORA ** -0.5),
        'gla_w_gf': nrm((L, GLA_GATE_RANK, GLA_HEADS * GLA_DK), GLA_GATE_RANK ** -0.5),
        'gla_b_gf': nrm((L, GLA_HEADS * GLA_DK), 0.1),
        'gla_w_gb': nrm((L, GLA_GATE_RANK, GLA_HEADS * GLA_DK), GLA_GATE_RANK ** -0.5),
        'gla_b_gb': nrm((L, GLA_HEADS * GLA_DK), 0.1),
        'gla_g_out': 1.0 + nrm((L, GLA_HEADS * GLA_DV), 0.05),
        'w_out': nrm((L, MIX_OUT, D), MIX_OUT ** -0.5),
        'w_router': nrm((L, D, E), D ** -0.5),
        'b_router': nrm((L, E), 0.01),
        'w_gu': nrm((L, E, D, 2 * F), D ** -0.5),
        'b_gu': nrm((L, E, 2 * F), 0.02),
        'w_down': nrm((L, E, F, D), F ** -0.5),
        'b_down': nrm((L, E, D), 0.02),
        'g_final': 1.0 + nrm((D,), 0.05),
    }


def reference(x_prompt, x_sample, c_prompt, c_sample, w_ada, b_ada, g_mix, g_ffn, w_in,
              mla_g_q, mla_g_kv, mla_w_uq, mla_w_ukv, gla_w_gf, gla_b_gf, gla_w_gb, gla_b_gb, gla_g_out,
              w_out, w_router, b_router, w_gu, b_gu, w_down, b_down, g_final):
    stacked = (w_ada, b_ada, g_mix, g_ffn, w_in, mla_g_q, mla_g_kv, mla_w_uq, mla_w_ukv,
               gla_w_gf, gla_b_gf, gla_w_gb, gla_b_gb, gla_g_out, w_out,
               w_router, b_router, w_gu, b_gu, w_down, b_down)

    def run(x, c):
        for l in range(DEPTH):
            x = encoder_layer(x, c, *[p[l] for p in stacked])
        return rms_norm(x, g_final)

    y_prompt = run(x_prompt, c_prompt)
    y_sample = run(x_sample, c_sample)
    return (y_prompt, y_sample)
```

```python
import contextlib
import numpy as np
import ml_dtypes
import concourse.bass as bass
import concourse.mybir as mybir
from concourse.bass_utils import run_bass_kernel_spmd

F32 = mybir.dt.float32
BF16 = mybir.dt.bfloat16
I32 = mybir.dt.int32
AF = mybir.ActivationFunctionType
ALU = mybir.AluOpType
AX = mybir.AxisListType

D = 1024
DIN = 2240
EPS = 1e-6
OFF = 4096

STRIDES = {}
LOCALS = {}


class Tok:
    def __init__(self, ctr, const, sym):
        self.ctr, self.const, self.sym = ctr, const, list(sym)

    def val(self):
        v = self.const
        for it, st in self.sym:
            if st:
                v = it * st + v
        return v


class Ctr:
    def __init__(self, name, sem, step):
        self.name, self.sem, self.step = name, sem, step
        self.base = OFF
        self.sym = []
        self.entry = []

    def bump(self):
        self.base += self.step
        return Tok(self, self.base, self.sym)

    def now(self):
        return Tok(self, self.base, self.sym)


class KB:
    def __init__(self, nc):
        self.nc = nc
        self.eng = {"pe": nc.tensor, "act": nc.scalar, "dve": nc.vector, "pool": nc.gpsimd, "sp": nc.sync}
        self.ctrs = {}
        self.loopstack = []
        self.es = None
        self.phase_id = 0

    @contextlib.contextmanager
    def phase(self, name, slots=()):
        nc = self.nc
        self.phase_id += 1
        self.pname = "%s%d" % (name, self.phase_id)
        with contextlib.ExitStack() as es:
            self.es = es
            self.ctrs = {}
            self.sw_free = list(self.swpool)
            for e in ("pe", "act", "dve", "pool"):
                self._mk(e, 1)
            for s in slots:
                self._mk(s, 16)
            for c in self.ctrs.values():
                nc.sync.sem_inc(c.sem, OFF)
            nc.all_engine_barrier()
            yield self
            for c in self.ctrs.values():
                if c.step == 16 and c.base > getattr(c, 'base0', OFF):
                    nc.sync.wait_ge(c.sem, c.base)
            nc.all_engine_barrier()
            for c in self.ctrs.values():
                self.eng.get(c.name, nc.sync).sem_clear(c.sem)
            nc.all_engine_barrier()
            for b_ in getattr(self, "global_bufs", []):
                b_.w, b_.r, b_.ld, b_.st = [], [], None, None
            self.es = None

    def _mk(self, name, step):
        sem = self.es.enter_context(self.nc.semaphore("%s_%s" % (self.pname, name)))
        self.ctrs[name] = Ctr(name, sem, step)

    def sb(self, name, shape, dt):
        return self.es.enter_context(self.nc.sbuf_tensor("%s_%s" % (self.pname, name), list(shape), dt))

    def ps(self, name, shape, dt):
        return self.es.enter_context(self.nc.psum_tensor("%s_%s" % (self.pname, name), list(shape), dt))

    def wait(self, e, deps):
        eng = self.eng[e]
        best = {}

        def add(d):
            if d is None:
                return
            if isinstance(d, (list, tuple)):
                for x in d:
                    add(x)
                return
            key = (d.ctr.name, tuple((id(i), st) for i, st in d.sym))
            if key not in best or d.const > best[key].const:
                best[key] = d

        add(list(deps))
        for d in best.values():
            eng.wait_ge(d.ctr.sem, d.val())

    def op(self, e, fn, deps=()):
        self.wait(e, deps)
        ins = fn(self.eng[e])
        c = self.ctrs[e]
        ins.then_inc(c.sem, 1)
        return c.bump()

    def dma(self, q, slot, out, in_, deps=(), **kw):
        self.wait(q, deps)
        ins = self.eng[q].dma_start(out=out, in_=in_, **kw)
        c = self.ctrs[slot]
        ins.then_inc(c.sem, 16)
        return c.bump()

    @contextlib.contextmanager
    def loop(self, name, n):
        name = "%s_%s" % (self.pname, name)
        st = STRIDES.setdefault(name, {})
        with self.nc.Fori(0, n) as it:
            for c in self.ctrs.values():
                c.entry.append(c.base)
                c.sym.append((it, st.get(c.name, 0)))
            self.loopstack.append(name)
            yield it
            self.loopstack.pop()
            for c in self.ctrs.values():
                e0 = c.entry.pop()
                c.sym.pop()
                actual = c.base - e0
                st[c.name] = actual
                c.base = e0 + actual * n

    def _reset_all(self, keep=()):
        nc = self.nc
        for c in self.ctrs.values():
            if c in keep:
                continue
            if c.step == 16 and c.base > 0:
                nc.sync.wait_ge(c.sem, c.base)
        nc.all_engine_barrier()
        for c in self.ctrs.values():
            if c in keep:
                continue
            self.eng.get(c.name, nc.sync).sem_clear(c.sem)
            c.base = 0
            c.base0 = 0
        nc.all_engine_barrier()

    def _init_regs(self):
        nc = self.nc
        names = {"Pool": "pool", "Activation": "act", "PE": "pe", "DVE": "dve", "SP": "sp"}
        self.lregs, self.lreg = [], []
        for lv in range(2):
            rr = nc.alloc_registers("lp%d" % lv, engines=mybir.ALL_ENGINES)
            self.lregs.append(rr)
            d = {}
            for h in rr.handles:
                for k_, v_ in names.items():
                    if k_ in str(h.engine):
                        d[v_] = h
            self.lreg.append(d)
        self.tregs = {e: [self.eng[e].alloc_register("t%d" % i) for i in range(4)] for e in ("pool", "sp", "pe", "act")}
        self.loop_id = 0

    @contextlib.contextmanager
    def reset_loop(self, n, level=0, keep=()):
        nc = self.nc
        if not hasattr(self, "lregs"):
            self._init_regs()
        self._reset_all(keep)
        self.loop_id += 1
        ls, le = "rl%d_loop" % self.loop_id, "rl%d_end" % self.loop_id
        regs = self.lregs[level]
        nc.regs_mov(regs, 0)
        nc.br(ls, engines=mybir.ALL_ENGINES)
        with nc.body(ls, valid_engines=mybir.ALL_ENGINES):
            yield self
            self._reset_all(keep)
            nc.regs_alu(regs, regs, 1, op=ALU.add)
            nc.br_lt(regs, n, on_true=ls, on_false=le, engines=mybir.ALL_ENGINES)
        nc.switch_bb(le)

    def dyn(self, e, slot, static_ap, terms, pre=None):
        eng = self.eng[e]
        r = self.tregs[e][slot]
        first = True
        if pre is not None:
            lv, add, mod, stride = pre
            eng.reg_add(r, self.lreg[lv][e], add)
            eng.reg_mod(r, r, mod)
            eng.reg_mul(r, r, stride)
            first = False
        t2 = self.tregs[e][3]
        for lv, stride in terms:
            if first:
                eng.reg_mul(r, self.lreg[lv][e], stride)
                first = False
            else:
                eng.reg_mul(t2, self.lreg[lv][e], stride)
                eng.reg_add(r, r, t2)
        eng.reg_add(r, r, int(static_ap.offset))
        return bass.AP(static_ap.tensor, r, [list(p_) for p_ in static_ap.ap])

    def mark(self, name, tok):
        key = "%s_%s" % (self.loopstack[-1], name)
        c = tok.ctr
        LOCALS[key] = (c.name, tok.const - c.entry[-1])
        return tok

    def prev(self, name, back=1):
        key = "%s_%s" % (self.loopstack[-1], name)
        if key not in LOCALS:
            return None
        cname, local = LOCALS[key]
        c = self.ctrs[cname]
        it, st = c.sym[-1]
        return Tok(c, c.entry[-1] + local - back * st, c.sym)


def bc_last(ap, n):
    return ap.unsqueeze(len(ap.shape)).to_broadcast(list(ap.shape) + [n])


class G:
    pass


C_QLAT, C_KVLAT, C_KROPE, C_FFT = 0, 256, 384, 416
C_GQ, C_GK, C_GV, C_GR, C_ZF, C_ZB = 672, 800, 928, 1184, 1440, 1456
C_DQ, C_DK, C_DV = 1472, 1728, 1984


def phase_mod(kb, g, l, s):
    with kb.phase("mod", slots=["ldw0", "ldw1", "ldb0", "ldb1", "ldc", "ldg"]):
        csb = kb.sb("c", [128, 8], F32)
        sg = kb.sb("sg", [128, 8], F32)
        sc = kb.sb("sc", [128, 8], F32)
        cbc = kb.sb("cbc", [128, 8, 128], BF16)
        wst = [kb.sb("wst%d" % i, [128, 8, 512], F32) for i in range(2)]
        wbf = [kb.sb("wbf%d" % i, [128, 8, 512], BF16) for i in range(2)]
        bst = [kb.sb("bst%d" % i, [128, 512], F32) for i in range(2)]
        gt = kb.sb("gt", [128, 2, 1024], F32)
        pm = [kb.ps("pm%d" % i, [128, 512], F32) for i in range(2)]
        t_c = kb.dma("sp", "ldc", csb[:], g.c_in[s])
        t_g0 = kb.dma("sp", "ldg", gt[:, 0, :], g.g_mix[l].partition_broadcast(128))
        t_g1 = kb.dma("sp", "ldg", gt[:, 1, :], g.g_ffn[l].partition_broadcast(128))
        t1 = kb.op("act", lambda e: e.activation(out=sg[:], in_=csb[:], func=AF.Sigmoid), [t_c])
        t2 = kb.op("dve", lambda e: e.tensor_mul(out=sc[:], in0=csb[:], in1=sg[:]), [t1])
        t3 = kb.op("dve", lambda e: e.tensor_copy(out=cbc[:], in_=bc_last(sc[:], 128)), [t2])
        free_w = [None, None]
        free_wbf = [None, None]
        free_p = [None, None]
        tes = []
        for n in range(12):
            p = n % 2
            tl = kb.dma("sp", "ldw%d" % p, wst[p][:],
                        g.w_ada[l, :, n * 512:(n + 1) * 512].rearrange("(k p) n -> p k n", p=128), [free_w[p]])
            tb = kb.dma("sp", "ldb%d" % p, bst[p][:], g.b_ada[l, n * 512:(n + 1) * 512].partition_broadcast(128),
                        [free_p[p]])
            tcst = kb.op("pool", lambda e: e.tensor_copy(out=wbf[p][:], in_=wst[p][:]), [tl, free_wbf[p]])
            free_w[p] = tcst
            for kc in range(8):
                tm = kb.op("pe", lambda e: e.matmul(pm[p][:], lhsT=cbc[:, kc, :], rhs=wbf[p][:, kc, :],
                                                    start=(kc == 0), stop=(kc == 7)),
                           [tcst, t3, free_p[p]] if kc == 0 else [])
            free_wbf[p] = tm
            te = kb.op("dve", lambda e: e.tensor_tensor(out=g.modbc[:, n // 2, (n % 2) * 512:(n % 2 + 1) * 512],
                                                        in0=pm[p][:], in1=bst[p][:], op=ALU.add), [tm, tb])
            free_p[p] = te
            tes.append(te)
        mbuf = g.modbc_buf
        mbuf.w = [tes[10], tes[11]]
        mbuf.r = []
        for sbk in range(g.seq_len[s] // 1024):
            sstore(kb, "act", mbuf, g.gtf[g.seq_off[s] // 1024 + sbk:g.seq_off[s] // 1024 + sbk + 1, :], g.modbc[0:1, 5, :])
        for j, gi, tg in ((1, 0, t_g1), (4, 1, t_g1)):
            kb.op("dve", lambda e: e.scalar_tensor_tensor(out=g.modbc[:, j, :], in0=g.modbc[:, j, :], scalar=1.0,
                                                          in1=gt[:, gi, :], op0=ALU.add, op1=ALU.mult),
                  [tes[2 * j], tes[2 * j + 1], tg])


def load_cast(kb, q, slot, dst, src, stage, deps=()):
    t = kb.dma(q, slot, stage, src, deps)
    return kb.op("pool", lambda e: e.tensor_copy(out=dst, in_=stage), [t])


def phase_a(kb, g, l, s):
    t0, S = g.seq_off[s], g.seq_len[s]
    src = g.x_in if l == 0 else g.xres
    with kb.phase("pa"):
        B = lambda nm, sh, dt, ps=False: mkbuf(kb, nm, sh, dt, ps)
        win = B("win", [128, 8, DIN], BF16)
        xt = [B("xt%d" % i, [128, D], F32) for i in range(2)]
        junk = B("junk", [128, D], BF16)
        tmp = B("tmp", [128, D], F32)
        hb = B("hb", [128, D], BF16)
        ss = B("ss", [128, 1], F32); rs = B("rs", [128, 1], F32)
        hT = [B("hT%d" % i, [128, 8, 128], BF16) for i in range(2)]
        zt = [B("zt%d" % i, [128, DIN], F32) for i in range(2)]
        pT = B("pT", [128, 8, 128], BF16, True)
        pz = B("pz", [128, 5, 512], F32, True)
        mb = g.modbc_buf
        wstg = [B("wstg%d" % i, [128, 8, 320], F32) for i in range(2)]
        for j in range(7):
            st_ = wstg[j % 2]
            sload(kb, "sp", st_, st_[:], g.w_in[l, :, j * 320:(j + 1) * 320].rearrange("(k p) n -> p k n", p=128))
            sop(kb, "pool" if j % 2 else "dve", lambda e: e.tensor_copy(out=win[:, :, j * 320:(j + 1) * 320], in_=st_[:]),
                reads=[st_], writes=[win])
        for i in range(S // 128):
            p = i % 2
            row = t0 + i * 128
            x = xt[p]
            sload(kb, "sp", x, x[:], src[row:row + 128, :])
            sop(kb, "act", lambda e: e.activation(out=junk[:], in_=x[:], func=AF.Square, accum_out=ss[:]),
                reads=[x], writes=[junk, ss])
            sop(kb, "dve", lambda e: e.tensor_scalar(out=rs[:], in0=ss[:], scalar1=1.0 / D, scalar2=EPS,
                                                     op0=ALU.mult, op1=ALU.add), reads=[ss], writes=[rs])
            sop(kb, "act", lambda e: e.activation(out=rs[:], in_=rs[:], func=AF.Ln), reads=[rs], writes=[rs])
            sop(kb, "act", lambda e: e.activation(out=rs[:], in_=rs[:], func=AF.Exp, scale=-0.5), reads=[rs], writes=[rs])
            sop(kb, "dve", lambda e: e.scalar_tensor_tensor(out=tmp[:], in0=x[:], scalar=rs[:], in1=g.modbc[:, 1, :],
                                                            op0=ALU.mult, op1=ALU.mult), reads=[x, rs, mb], writes=[tmp])
            sop(kb, "pool", lambda e: e.tensor_tensor(out=hb[:], in0=tmp[:], in1=g.modbc[:, 0, :], op=ALU.add),
                reads=[tmp, mb], writes=[hb])
            transposes(kb, g, hb, 8, pT, hT[p])
            for nb in range(5):
                w = 512 if nb < 4 else DIN - 2048
                for kc in range(8):
                    sop(kb, "pe", lambda e: e.matmul(pz[:, nb, 0:w], lhsT=hT[p][:, kc, :], rhs=win[:, kc, nb * 512:nb * 512 + w],
                                                     start=(kc == 0), stop=(kc == 7)), reads=[hT[p], win], writes=[pz])
            z = zt[p]
            pre = list(z.w) + list(z.r)
            ta = sop(kb, "act", lambda e: e.copy(out=z[:, 0:1024], in_=pz[:, 0:2, :].rearrange("p a b -> p (a b)")),
                     reads=[pz], extra=pre)
            sop(kb, "dve", lambda e: e.tensor_copy(out=z[:, 1024:2048], in_=pz[:, 2:4, :].rearrange("p a b -> p (a b)")),
                reads=[pz], extra=pre)
            tb = sop(kb, "dve", lambda e: e.tensor_copy(out=z[:, 2048:DIN], in_=pz[:, 4, 0:DIN - 2048]), reads=[pz], extra=pre)
            z.w, z.r = [ta, tb], []
            sstore(kb, "act", z, g.zs[row:row + 128, :], z[:])


W_SPECS = [
    ("w_ada", [2, 1024, 6144]), ("b_ada", [2, 6144]), ("g_mix", [2, 1024]), ("g_ffn", [2, 1024]),
    ("w_in", [2, 1024, 2240]), ("mla_g_q", [2, 256]), ("mla_g_kv", [2, 128]), ("mla_w_uq", [2, 256, 384]),
    ("mla_w_ukv", [2, 128, 512]), ("gla_w_gf", [2, 16, 128]), ("gla_b_gf", [2, 128]), ("gla_w_gb", [2, 16, 128]),
    ("gla_b_gb", [2, 128]), ("gla_g_out", [2, 256]), ("w_out", [2, 1024, 1024]), ("w_router", [2, 1024, 32]),
    ("b_router", [2, 32]), ("w_gu", [2, 32, 1024, 2048]), ("b_gu", [2, 32, 2048]), ("w_down", [2, 32, 1024, 1024]),
    ("b_down", [2, 32, 1024]), ("g_final", [1024]),
]


def build_once(seq_lens, depth=2, n_exp=32, stop=None, dbg=(), dft_n=None, static_moe=True):
    nc = bass.Bass("TRN2", target_bir_lowering=False)
    g = G()
    g.seq_len = list(seq_lens)
    g.seq_off = [int(x) for x in np.cumsum([0] + list(seq_lens))[:-1]]
    T = int(sum(seq_lens))
    g.T = T
    ns = len(seq_lens)

    def din(name, shape, dt=F32):
        return nc.dram_tensor(name, list(shape), dt, kind="ExternalInput").ap()

    def dscr(name, shape, dt=F32):
        kind = "ExternalOutput" if name in dbg else "Internal"
        return nc.dram_tensor(name, list(shape), dt, kind=kind).ap()

    g.x_in = din("x_in", [T, D])
    g.c_in = din("c_in", [ns, 128, 8])
    for name, shape in W_SPECS:
        shape = list(shape)
        if name in ("w_gu", "b_gu", "w_down", "b_down"):
            shape[1] = n_exp
        setattr(g, name, din(name, shape))
    g.n_exp = n_exp
    g.static_moe = static_moe
    g.b_gu_l = din("b_gu_l", [2, n_exp, 128, 16])
    g.y_out = nc.dram_tensor("y_out", [T, D], F32, kind="ExternalOutput").ap()
    g.xres = dscr("xres", [T, D])
    g.zs = dscr("zs", [T, DIN])
    g.modd = dscr("modd", [128, 6, 1024])
    g.dft_n = dft_n or max(seq_lens)
    Smax = max(seq_lens)
    g.Smax = Smax
    for name, (shape, dt) in const_specs(Smax).items():
        setattr(g, name, din(name, shape, dt))
    g.mq = dscr("mq", [4, 96, Smax], BF16)
    g.mk = dscr("mk", [4, 96, Smax], BF16)
    g.mv = dscr("mv", [Smax, 4, 65], BF16)
    g.dq = dscr("dq", [4, 64, Smax], BF16)
    g.dk = dscr("dk", [4, 64, Smax], BF16)
    g.dv = dscr("dv", [Smax, 4, 65], BF16)
    g.oT = dscr("oT", [1024, Smax], BF16)
    g.go = dscr("go", [Smax, 256], F32)
    g.h2T = dscr("h2T", [128, 8, T], BF16)
    g.gates = dscr("gates", [T, 32], F32)
    g.gtf = dscr("gtf", [T // 1024, 1024], F32)

    kb = KB(nc)
    with contextlib.ExitStack() as ges:
        g.modbc = ges.enter_context(nc.sbuf_tensor("modbc", [128, 6, 1024], F32))
        g.ident = ges.enter_context(nc.sbuf_tensor("ident", [128, 128], BF16))
        identf = ges.enter_context(nc.sbuf_tensor("identf", [128, 128], F32))
        g.identb = g.ident
        kb.swpool = [ges.enter_context(nc.semaphore("swq%d" % i)) for i in range(8)]
        g.modbc_buf = Buf(kb, g.modbc, "modbc")
        kb.global_bufs = [g.modbc_buf]
        g.identf = identf
        with kb.phase("init"):
            t = kb.op("pool", lambda e: e.memset(identf[:], 0.0))
            t = kb.op("pool", lambda e: e.affine_select(out=identf[:], in_=identf[:], pattern=[[-1, 128]],
                                                        compare_op=ALU.not_equal, fill=1.0, base=0,
                                                        channel_multiplier=1), [t])
            kb.op("pool", lambda e: e.tensor_copy(out=g.ident[:], in_=identf[:]), [t])
        for l in range(depth):
            for s in range(ns):
                phase_mod(kb, g, l, s)
                if "modd" in dbg:
                    with kb.phase("dbg", slots=["st"]):
                        t = kb.dma("sp", "st", g.modd, g.modbc[:])
                        kb.wait("sp", [t])
                if stop == "mod":
                    break
                if stop == "conly":
                    phase_c(kb, g, l, s)
                    break
                phase_a(kb, g, l, s)
                if stop == "a":
                    break
                phase_mla_prep(kb, g, l, s)
                phase_attn(kb, g, g.mq, g.mk, g.mv, 96, g.seq_len[s], 96 ** -0.5, 0)
                if stop == "mla":
                    break
                phase_dil_prep(kb, g, l, s)
                phase_attn(kb, g, g.dq, g.dk, g.dv, 64, g.seq_len[s], 0.125, 768, masks=g.dil_mask)
                phase_fft(kb, g, l, s)
                if stop == "fft":
                    break
                phase_gla(kb, g, l, s)
                if stop == "gla":
                    break
                phase_c(kb, g, l, s)
                if stop == "c":
                    break
            if stop:
                break
            phase_moe(kb, g, l)
        if not stop:
            phase_final(kb, g)
    return nc


def build(*a, **kw):
    STRIDES.clear()
    LOCALS.clear()
    return build_once(*a, **kw)


class Buf:
    def __init__(self, kb, t, name):
        self.t, self.name, self.kb = t, name, kb
        self.w = []
        self.r = []
        self.ld = None
        self.st = None

    def __getitem__(self, k):
        return self.t[k]


class View:
    def __init__(self, parent, ap, name):
        self.__dict__["p"] = parent
        self.__dict__["t"] = ap
        self.__dict__["name"] = name

    def __getitem__(self, k):
        return self.t[k]

    def __getattr__(self, k):
        return getattr(self.p, k)

    def __setattr__(self, k, v):
        setattr(self.p, k, v)


def _static_base(kb, name, sw=False):
    if sw:
        kb.ctrs[name] = Ctr(name, kb.sw_free.pop(), 16)
    else:
        kb._mk(name, 16)
    c = kb.ctrs[name]
    c.base = 0
    c.base0 = 0
    return c


def mkbuf(kb, name, shape, dt, psum=False):
    t = kb.ps(name, shape, dt) if psum else kb.sb(name, shape, dt)
    return Buf(kb, t, name)


def sop(kb, e, fn, reads=(), writes=(), extra=()):
    deps = list(extra)
    for b in reads:
        deps += b.w
    for b in writes:
        deps += b.w + b.r
    if e == "pe":
        deps = [d for d in deps if d is not None and d.ctr.name != "pe"]
    tok = kb.op(e, fn, deps)
    for b in reads:
        b.r = [t for t in b.r if not (t.ctr is tok.ctr and len(t.sym) == len(tok.sym))] + [tok]
    for b in writes:
        b.w = [tok]
        b.r = []
    return tok


def sload(kb, q, buf, out_ap, in_ap, group=False, **kw):
    if buf.ld is None:
        buf.ld = _static_base(kb, buf.name + "_ld", sw=(q == "pool"))
    deps = [] if group else (buf.w + buf.r)
    kb.wait(q, deps)
    ins = kb.eng[q].dma_start(out=out_ap, in_=in_ap, **kw)
    ins.then_inc(buf.ld.sem, 16)
    tok = buf.ld.bump()
    buf.w = [tok]
    if not group:
        buf.r = []
    return tok


def sstore(kb, q, buf, out_ap, in_ap):
    if buf.st is None:
        buf.st = _static_base(kb, buf.name + "_st")
    kb.wait(q, buf.w)
    ins = kb.eng[q].dma_start(out=out_ap, in_=in_ap)
    ins.then_inc(buf.st.sem, 16)
    tok = buf.st.bump()
    buf.r = [t for t in buf.r if t.ctr is not buf.st] + [tok]
    return tok


def transposes(kb, g, src, n, pT, dst, eng="act", w=128, ident=None):
    ident = ident if ident is not None else g.identb
    for j in range(n):
        sop(kb, "pe", lambda e: e.transpose(out=pT[0:w, j, :], in_=src[:, j * w:(j + 1) * w], identity=ident[:]),
            reads=[src], writes=[pT])
    if eng == "act":
        sop(kb, "act", lambda e: e.copy(out=dst[0:w, 0:n, :], in_=pT[0:w, 0:n, :]), reads=[pT], writes=[dst])
    else:
        sop(kb, eng, lambda e: e.tensor_copy(out=dst[0:w, 0:n, :], in_=pT[0:w, 0:n, :]), reads=[pT], writes=[dst])


def rms_cols(kb, src, c0, n, ss, rs, junk):
    sop(kb, "act", lambda e: e.activation(out=junk[:, 0:n], in_=src[:, c0:c0 + n], func=AF.Square, accum_out=ss[:]),
        reads=[src], writes=[junk, ss])
    sop(kb, "dve", lambda e: e.tensor_scalar(out=rs[:], in0=ss[:], scalar1=1.0 / n, scalar2=EPS,
                                             op0=ALU.mult, op1=ALU.add), reads=[ss], writes=[rs])
    sop(kb, "act", lambda e: e.activation(out=rs[:], in_=rs[:], func=AF.Ln), reads=[rs], writes=[rs])
    sop(kb, "act", lambda e: e.activation(out=rs[:], in_=rs[:], func=AF.Exp, scale=-0.5), reads=[rs], writes=[rs])


def rope_apply(kb, g, psrc, rows, width, rotT, cosb, sinb, sb16, prot, t1, t2, outb):
    sop(kb, "act", lambda e: e.copy(out=sb16[0:rows, 0:width], in_=psrc[0:rows, 0:width]), reads=[psrc], writes=[sb16])
    sop(kb, "pe", lambda e: e.matmul(prot[0:rows, 0:width], lhsT=rotT[0:rows, 0:rows], rhs=sb16[0:rows, 0:width],
                                     start=True, stop=True), reads=[sb16, rotT], writes=[prot])
    cos_ap, sin_ap = cosb
    sop(kb, "dve", lambda e: e.tensor_tensor(out=t1[0:rows, 0:width], in0=sb16[0:rows, 0:width], in1=cos_ap, op=ALU.mult),
        reads=[sb16, sinb], writes=[t1])
    sop(kb, "dve", lambda e: e.tensor_tensor(out=t2[0:rows, 0:width], in0=prot[0:rows, 0:width], in1=sin_ap, op=ALU.mult),
        reads=[prot, sinb], writes=[t2])
    sop(kb, "pool", lambda e: e.tensor_tensor(out=outb[0:rows, 0:width], in0=t1[0:rows, 0:width], in1=t2[0:rows, 0:width],
                                              op=ALU.add), reads=[t1, t2], writes=[outb])


def phase_mla_prep(kb, g, l, s):
    t0, S = g.seq_off[s], g.seq_len[s]
    with kb.phase("mp"):
        B = lambda n, sh, dt, ps=False: mkbuf(kb, n, sh, dt, ps)
        wst = B("wst", [128, 2, 512], F32)
        wuq = B("wuq", [128, 2, 384], BF16)
        wk = B("wk", [128, 4, 96], BF16)
        wv = B("wv", [128, 256], BF16)
        sel = B("sel", [32, 96], BF16)
        gq = B("gq", [128, 384], F32)
        rot = B("rot", [96, 96], BF16)
        zt = [B("zt%d" % i, [128, 416], F32) for i in range(2)]
        cs = [B("cs%d" % i, [96, 2, 128], F32) for i in range(2)]
        junk = B("junk", [128, 256], BF16)
        ss = B("ss", [128, 1], F32); rs = B("rs", [128, 1], F32)
        ss2 = B("ss2", [128, 1], F32); rs2 = B("rs2", [128, 1], F32)
        nb = B("nb", [128, 512], BF16)
        nT = B("nT", [128, 4, 128], BF16)
        pT = B("pT", [128, 4, 128], BF16, True)
        pq = B("pq", [96, 512], F32, True)
        pk = B("pk", [96, 512], F32, True)
        prot = B("prot", [96, 512], F32, True)
        pv = B("pv", [128, 256], F32, True)
        sb16 = B("sb16", [96, 512], BF16)
        t1 = B("t1", [96, 512], F32); t2 = B("t2", [96, 512], F32)
        qo = [B("qo%d" % i, [96, 512], BF16) for i in range(2)]
        ko = [B("ko%d" % i, [96, 512], BF16) for i in range(2)]
        vo = [B("vo%d" % i, [128, 4, 65], BF16) for i in range(2)]
        sload(kb, "sp", wst, wst[:, :, 0:384], g.mla_w_uq[l].rearrange("(k p) n -> p k n", p=128))
        sop(kb, "pool", lambda e: e.tensor_copy(out=wuq[:], in_=wst[:, :, 0:384]), reads=[wst], writes=[wuq])
        sload(kb, "sp", wst, wst[:, 0, :], g.mla_w_ukv[l])
        sop(kb, "pool", lambda e: e.memset(wk[:], 0.0), writes=[wk])
        sop(kb, "pool", lambda e: e.tensor_copy(out=wk[:, :, 0:64],
                                                in_=wst[:, 0, :].rearrange("p (h c) -> p h c", h=4)[:, :, 0:64]),
            reads=[wst], writes=[wk])
        sop(kb, "pool", lambda e: e.tensor_copy(out=wv[:].rearrange("p (h c) -> p h c", h=4),
                                                in_=wst[:, 0, :].rearrange("p (h c) -> p h c", h=4)[:, :, 64:128]),
            reads=[wst], writes=[wv])
        sop(kb, "pool", lambda e: e.memset(sel[:], 0.0), writes=[sel])
        sop(kb, "pool", lambda e: e.tensor_copy(out=sel[:, 64:96], in_=g.identb[0:32, 0:32]), writes=[sel])
        sload(kb, "sp", gq, gq[:, 0:256], g.mla_g_q[l].partition_broadcast(128))
        sload(kb, "sp", gq, gq[:, 256:384], g.mla_g_kv[l].partition_broadcast(128), group=True)
        sload(kb, "sp", rot, rot[:], g.rot96T)
        for p in range(2):
            sop(kb, "pool", lambda e: e.memset(vo[p][:, :, 64:65], 1.0), writes=[vo[p]])
        for i in range(S // 128):
            p = i % 2
            row = t0 + i * 128
            z = zt[p]
            sload(kb, "sp", z, z[:], g.zs[row:row + 128, 0:416])
            sload(kb, "sp", cs[p], cs[p][:], g.rope_mla[:, :, i * 128:(i + 1) * 128].rearrange("c d t -> d c t"))
            rms_cols(kb, z, 0, 256, ss, rs, junk)
            rms_cols(kb, z, 256, 128, ss2, rs2, junk)
            sop(kb, "dve", lambda e: e.scalar_tensor_tensor(out=nb[:, 0:256], in0=z[:, 0:256], scalar=rs[:],
                                                            in1=gq[:, 0:256], op0=ALU.mult, op1=ALU.mult),
                reads=[z, rs, gq], writes=[nb])
            sop(kb, "dve", lambda e: e.scalar_tensor_tensor(out=nb[:, 256:384], in0=z[:, 256:384], scalar=rs2[:],
                                                            in1=gq[:, 256:384], op0=ALU.mult, op1=ALU.mult),
                reads=[z, rs2, gq], writes=[nb])
            sop(kb, "pool", lambda e: e.tensor_copy(out=nb[:, 384:416], in_=z[:, 384:416]), reads=[z], writes=[nb])
            sop(kb, "pool", lambda e: e.memset(nb[:, 416:512], 0.0), writes=[nb])
            transposes(kb, g, nb, 4, pT, nT)
            for h in range(4):
                for c in range(2):
                    sop(kb, "pe", lambda e: e.matmul(pq[:, h * 128:(h + 1) * 128], lhsT=wuq[:, c, h * 96:(h + 1) * 96],
                                                     rhs=nT[:, c, :], start=(c == 0), stop=(c == 1)),
                        reads=[wuq, nT], writes=[pq])
            for h in range(4):
                sop(kb, "pe", lambda e: e.matmul(pk[:, h * 128:(h + 1) * 128], lhsT=wk[:, h, :], rhs=nT[:, 2, :],
                                                 start=True, stop=False), reads=[wk, nT], writes=[pk])
                sop(kb, "pe", lambda e: e.matmul(pk[:, h * 128:(h + 1) * 128], lhsT=sel[:], rhs=nT[0:32, 3, :],
                                                 start=False, stop=True), reads=[sel, nT], writes=[pk])
            sop(kb, "pe", lambda e: e.matmul(pv[:], lhsT=nT[:, 2, :], rhs=wv[:], start=True, stop=True),
                reads=[nT, wv], writes=[pv])
            cosb = cs[p][:, 0, :].unsqueeze(1).to_broadcast([96, 4, 128])
            sinb = cs[p][:, 1, :].unsqueeze(1).to_broadcast([96, 4, 128])
            v4 = lambda b: b
            for (src, dst) in ((pq, qo[p]), (pk, ko[p])):
                rope_apply4(kb, g, src, rot, cs[p], sb16, prot, t1, t2, dst)
            sop(kb, "act", lambda e: e.copy(out=vo[p][:, :, 0:64], in_=pv[:].rearrange("p (h c) -> p h c", h=4)),
                reads=[pv], writes=[vo[p]])
            sstore(kb, "act", qo[p], g.mq[:, :, i * 128:(i + 1) * 128].rearrange("h d t -> d h t"),
                   qo[p][:].rearrange("d (h t) -> d h t", h=4))
            sstore(kb, "act", ko[p], g.mk[:, :, i * 128:(i + 1) * 128].rearrange("h d t -> d h t"),
                   ko[p][:].rearrange("d (h t) -> d h t", h=4))
            sstore(kb, "act", vo[p], g.mv[i * 128:(i + 1) * 128, :, :], vo[p][:])


def rope_apply4(kb, g, psrc, rot, csb, sb16, prot, t1, t2, outb, rows=96):
    sop(kb, "act", lambda e: e.copy(out=sb16[0:rows, :], in_=psrc[0:rows, :]), reads=[psrc], writes=[sb16])
    sop(kb, "pe", lambda e: e.matmul(prot[0:rows, :], lhsT=rot[0:rows, 0:rows], rhs=sb16[0:rows, :],
                                     start=True, stop=True), reads=[sb16, rot], writes=[prot])
    cosb = csb[0:rows, 0, :].unsqueeze(1).to_broadcast([rows, 4, 128])
    sinb = csb[0:rows, 1, :].unsqueeze(1).to_broadcast([rows, 4, 128])
    v = lambda b: b[0:rows, :].rearrange("d (h t) -> d h t", h=4)
    sop(kb, "dve", lambda e: e.tensor_tensor(out=v(t1), in0=v(sb16), in1=cosb, op=ALU.mult),
        reads=[sb16, csb], writes=[t1])
    sop(kb, "dve", lambda e: e.tensor_tensor(out=v(t2), in0=v(prot), in1=sinb, op=ALU.mult),
        reads=[prot, csb], writes=[t2])
    sop(kb, "pool", lambda e: e.tensor_tensor(out=outb[0:rows, :], in0=t1[0:rows, :], in1=t2[0:rows, :], op=ALU.add),
        reads=[t1, t2], writes=[outb])


def phase_attn(kb, g, qd, kd, vd, dk, S, scale, orow0, masks=None):
    nkt = S // 128
    for h in range(4):
        with kb.phase("at"):
            B = lambda n, sh, dt, ps=False: mkbuf(kb, n, sh, dt, ps)
            kT = B("kT", [dk, S], BF16)
            vv = B("vv", [128, nkt, 65], BF16)
            qb = [B("qb%d" % i, [dk, 512], BF16) for i in range(2)]
            pS = [B("pS%d" % i, [128, 512], F32, True) for i in range(2)]
            pO = [B("pO%d" % i, [65, 512], F32, True) for i in range(2)]
            pB = B("pB", [64, 512], F32, True)
            pt = [B("pt%d" % i, [128, 512], BF16) for i in range(3)]
            osb = B("osb", [65, 512], F32)
            rd = B("rd", [65, 512], F32)
            ob = [B("ob%d" % i, [64, 512], BF16) for i in range(2)]
            ones = B("ones", [65, 64], F32)
            mk = None
            if masks is not None:
                mk = B("mk", [128, 20, 512], BF16)
                sload(kb, "sp", mk, mk[:], masks)
            sop(kb, "pool", lambda e: e.memset(ones[:], 1.0), writes=[ones])
            sload(kb, "sp", kT, kT[:], kd[h, :, 0:S])
            for v0 in range(0, nkt, 16):
                v1 = min(nkt, v0 + 16)
                sload(kb, "sp", vv, vv[:, v0:v1, :], vd[v0 * 128:v1 * 128, h, :].rearrange("(t p) e -> p t e", p=128),
                      group=(v0 > 0))
            cnt = 0
            for qi in range(S // 512):
                p = qi % 2
                sload(kb, "sp", qb[p], qb[p][:], qd[h, :, qi * 512:(qi + 1) * 512])
                if masks is None:
                    kts = list(range(nkt))
                else:
                    kts = [kt for kt in range(4 * qi - 8, 4 * qi + 12) if 0 <= kt < nkt]
                for j, kt in enumerate(kts):
                    ps_ = pS[cnt % 2]
                    ptb = pt[cnt % 3]
                    cnt += 1
                    sop(kb, "pe", lambda e: e.matmul(ps_[:], lhsT=kT[:, kt * 128:(kt + 1) * 128], rhs=qb[p][:],
                                                     start=True, stop=True), reads=[kT, qb[p]], writes=[ps_])
                    sop(kb, "act", lambda e: e.activation(out=ptb[:], in_=ps_[:], func=AF.Exp, scale=scale),
                        reads=[ps_], writes=[ptb])
                    if mk is not None:
                        mi = kt - (4 * qi - 8)
                        sop(kb, "dve" if cnt % 2 else "pool",
                            lambda e: e.tensor_tensor(out=ptb[:], in0=ptb[:], in1=mk[:, mi, :], op=ALU.mult),
                            reads=[ptb, mk], writes=[ptb])
                    sop(kb, "pe", lambda e: e.matmul(pO[p][:], lhsT=vv[:, kt, :], rhs=ptb[:],
                                                     start=(j == 0), stop=(j == len(kts) - 1)),
                        reads=[vv, ptb], writes=[pO[p]])
                sop(kb, "act", lambda e: e.copy(out=osb[:], in_=pO[p][:]), reads=[pO[p]], writes=[osb])
                sop(kb, "dve", lambda e: e.reciprocal(out=rd[64:65, :], in_=osb[64:65, :]), reads=[osb], writes=[rd])
                sop(kb, "pe", lambda e: e.matmul(pB[:], lhsT=ones[64:65, :], rhs=rd[64:65, :], start=True, stop=True),
                    reads=[ones, rd], writes=[pB])
                sop(kb, "dve", lambda e: e.tensor_tensor(out=ob[p][:], in0=osb[0:64, :], in1=pB[:], op=ALU.mult),
                    reads=[osb, pB], writes=[ob[p]])
                sstore(kb, "act", ob[p], g.oT[orow0 + h * 64:orow0 + (h + 1) * 64, qi * 512:(qi + 1) * 512], ob[p][:])


def const_specs(Smax):
    return {
        "rot96T": ([96, 96], BF16),
        "rope_mla": ([2, 96, Smax], F32),
        "rot128T": ([128, 128], BF16),
        "rope_dil": ([2, 128, Smax], F32),
        "dil_mask": ([128, 20, 512], BF16),
        "c64bd": ([128, 2, 128], BF16),
        "gla_mats": ([128, 2, 3, 128], BF16),
        "gla_cm": ([128, 2, 128], F32),
        "gla_hm": ([128, 4], F32),
        "gla_bd": ([128, 256], F32),
        "dftc": ([Smax, Smax], BF16),
        "dfts": ([Smax, Smax], BF16),
    }


def make_consts(Smax):
    bf = ml_dtypes.bfloat16
    c = {}
    R = np.zeros((96, 96), np.float32)
    for i in range(16):
        R[64 + i, 80 + i] = -1.0
        R[80 + i, 64 + i] = 1.0
    c["rot96T"] = R.T.astype(bf)
    pos = np.arange(Smax, dtype=np.float32)
    inv = np.power(np.float32(10000.0), -np.arange(0, 32, 2, dtype=np.float32) / 32)
    ang = pos[None, :] * inv[:, None]
    rm = np.zeros((2, 96, Smax), np.float32)
    rm[0, :64] = 1.0
    rm[0, 64:80] = np.cos(ang); rm[0, 80:96] = np.cos(ang)
    rm[1, 64:80] = np.sin(ang); rm[1, 80:96] = np.sin(ang)
    c["rope_mla"] = rm
    R = np.zeros((128, 128), np.float32)
    for hh in range(2):
        for i in range(8):
            R[hh * 64 + i, hh * 64 + 8 + i] = -1.0
            R[hh * 64 + 8 + i, hh * 64 + i] = 1.0
    c["rot128T"] = R.T.astype(bf)
    inv = np.power(np.float32(500000.0), -np.arange(0, 16, 2, dtype=np.float32) / 16)
    ang = pos[None, :] * inv[:, None]
    rd = np.zeros((2, 128, Smax), np.float32)
    rd[0] = 1.0
    for hh in range(2):
        rd[0, hh * 64:hh * 64 + 8] = np.cos(ang); rd[0, hh * 64 + 8:hh * 64 + 16] = np.cos(ang)
        rd[1, hh * 64:hh * 64 + 8] = np.sin(ang); rd[1, hh * 64 + 8:hh * 64 + 16] = np.sin(ang)
    c["rope_dil"] = rd
    kk = np.arange(128)[:, None, None]; mi = np.arange(20)[None, :, None]; qq = np.arange(512)[None, None, :]
    d = (mi - 8) * 128 + kk - qq
    m = (np.abs(d) <= 64).astype(np.float32) + ((d % 4 == 0) & (np.abs(d) <= 256)) + ((d % 16 == 0) & (np.abs(d) <= 1024))
    c["dil_mask"] = m.astype(bf)
    n = np.arange(64)
    a = 2 * np.pi * ((n[:, None] * n[None, :]) % 64) / 64
    cb = np.zeros((128, 2, 128), np.float32)
    for hh in range(2):
        cb[hh * 64:(hh + 1) * 64, 0, hh * 64:(hh + 1) * 64] = np.cos(a)
        cb[hh * 64:(hh + 1) * 64, 1, hh * 64:(hh + 1) * 64] = -np.sin(a)
    c["c64bd"] = cb.astype(bf)
    tp = np.arange(128)[:, None]; tt = np.arange(128)[None, :]
    gm = np.zeros((128, 2, 3, 128), np.float32)
    gc = np.zeros((128, 2, 128), np.float32)
    for d_ in range(2):
        Lc = (tp <= tt) if d_ == 0 else (tp >= tt)
        Lmid = np.broadcast_to((tp <= 64) if d_ == 0 else (tp >= 64), (128, 128))
        Lc = Lc.astype(np.float32); Lmid = Lmid.astype(np.float32)
        gm[:, d_, 0] = (Lc - Lmid) * (-1.0 / 16)
        gm[:, d_, 1] = (1.0 - Lc) * (-1.0 / 16)
        gm[:, d_, 2] = Lc * (-1.0 / 16)
        gc[:, d_] = Lc
    c["gla_mats"] = gm.astype(bf)
    c["gla_cm"] = gc
    c["gla_hm"] = (np.arange(128)[:, None] // 32 == np.arange(4)[None, :]).astype(np.float32)
    c["gla_bd"] = (np.arange(128)[:, None] // 32 == np.arange(256)[None, :] // 64).astype(np.float32)
    n = np.arange(Smax, dtype=np.int64)
    a = (2 * np.pi / Smax) * ((n[:, None] * n[None, :]) % Smax).astype(np.float64)
    c["dftc"] = np.cos(a).astype(np.float32).astype(bf)
    c["dfts"] = np.sin(a).astype(np.float32).astype(bf)
    return c


def phase_dil_prep(kb, g, l, s):
    t0, S = g.seq_off[s], g.seq_len[s]
    with kb.phase("dp"):
        B = lambda n, sh, dt, ps=False: mkbuf(kb, n, sh, dt, ps)
        rot = B("rot", [128, 128], BF16)
        zt = [B("zt%d" % i, [128, 768], F32) for i in range(2)]
        cs = [B("cs%d" % i, [128, 2, 128], F32) for i in range(2)]
        nb = B("nb", [128, 512], BF16)
        pT = B("pT", [128, 512], BF16, True)
        prot = B("prot", [128, 512], F32, True)
        sb16 = B("sb16", [128, 512], BF16)
        t1 = B("t1", [128, 512], F32); t2 = B("t2", [128, 512], F32)
        qk = [B("qk%d" % i, [128, 512], BF16) for i in range(2)]
        vo = [B("vo%d" % i, [128, 4, 65], BF16) for i in range(2)]
        sload(kb, "sp", rot, rot[:], g.rot128T)
        for p in range(2):
            sop(kb, "pool", lambda e: e.memset(vo[p][:, :, 64:65], 1.0), writes=[vo[p]])
        for i in range(S // 128):
            p = i % 2
            row = t0 + i * 128
            z = zt[p]
            sload(kb, "sp", z, z[:], g.zs[row:row + 128, C_DQ:C_DQ + 768])
            sload(kb, "sp", cs[p], cs[p][:], g.rope_dil[:, :, i * 128:(i + 1) * 128].rearrange("c d t -> d c t"))
            sop(kb, "dve", lambda e: e.tensor_copy(out=nb[:], in_=z[:, 0:512]), reads=[z], writes=[nb])
            for j in range(4):
                sop(kb, "pe", lambda e: e.transpose(out=pT[:, j * 128:(j + 1) * 128], in_=nb[:, j * 128:(j + 1) * 128],
                                                    identity=g.identb[:]), reads=[nb], writes=[pT])
            rope_apply4(kb, g, pT, rot, cs[p], sb16, prot, t1, t2, qk[p], rows=128)
            sop(kb, "act", lambda e: e.copy(out=vo[p][:, :, 0:64], in_=z[:, 512:768].rearrange("p (h c) -> p h c", h=4)),
                reads=[z], writes=[vo[p]])
            sstore(kb, "act", qk[p], g.dq[:, :, i * 128:(i + 1) * 128].rearrange("(b a) d t -> (a d) b t", a=2),
                   qk[p][:, 0:256].rearrange("p (b t) -> p b t", b=2))
            sstore(kb, "act", qk[p], g.dk[:, :, i * 128:(i + 1) * 128].rearrange("(b a) d t -> (a d) b t", a=2),
                   qk[p][:, 256:512].rearrange("p (b t) -> p b t", b=2))
            sstore(kb, "act", vo[p], g.dv[i * 128:(i + 1) * 128, :, :], vo[p][:])


def phase_fft(kb, g, l, s):
    t0, S = g.seq_off[s], g.seq_len[s]
    nt_ = S // 128
    r = g.dft_n // S
    with kb.phase("ff"):
        B = lambda n, sh, dt, ps=False: mkbuf(kb, n, sh, dt, ps)
        cb = B("cb", [128, 2, 128], BF16)
        zt = [B("zt%d" % i, [128, 256], F32) for i in range(2)]
        nb = B("nb", [128, 256], BF16)
        pT = B("pT", [128, 2, 128], BF16, True)
        uT = B("uT", [128, 2, 128], BF16)
        pU = B("pU", [128, 2, 256], F32, True)
        UC = B("UC", [128, nt_, 2, 256], BF16)
        ct = [B("ct%d" % i, [128, 2, 512], BF16) for i in range(3)]
        pY = [B("pY%d" % i, [128, 512], F32, True) for i in range(4)]
        ob = [B("ob%d" % i, [128, 512], BF16) for i in range(2)]
        sload(kb, "sp", cb, cb[:], g.c64bd)
        for i in range(nt_):
            p = i % 2
            row = t0 + i * 128
            z = zt[p]
            sload(kb, "sp", z, z[:], g.zs[row:row + 128, C_FFT:C_FFT + 256])
            sop(kb, "dve", lambda e: e.tensor_copy(out=nb[:], in_=z[:]), reads=[z], writes=[nb])
            transposes(kb, g, nb, 2, pT, uT)
            for cs_ in range(2):
                for c in range(2):
                    sop(kb, "pe", lambda e: e.matmul(pU[:, cs_, c * 128:(c + 1) * 128], lhsT=uT[:, c, :], rhs=cb[:, cs_, :],
                                                     start=True, stop=True), reads=[uT, cb], writes=[pU])
            sop(kb, "act", lambda e: e.copy(out=UC[:, i, :, :], in_=pU[:]), reads=[pU], writes=[])
        UC.w = [kb.ctrs["act"].now()]
        scale = float((64.0 * S) ** -0.5)
        cnt = 0
        for kb_ in range(S // 512):
            pp = kb_ % 2
            for nt in range(nt_):
                t = ct[cnt % 3]
                cnt += 1
                for cs_, tab in ((0, g.dftc), (1, g.dfts)):
                    sload(kb, "sp", t, t[:, cs_, :],
                          tab.rearrange("(n r) k -> n r k", r=r)[nt * 128:(nt + 1) * 128, 0, kb_ * 512:(kb_ + 1) * 512],
                          group=(cs_ == 1))
                for c in range(2):
                    for cs_ in range(2):
                        sop(kb, "pe", lambda e: e.matmul(pY[pp * 2 + c][:], lhsT=UC[:, nt, cs_, c * 128:(c + 1) * 128],
                                                         rhs=t[:, cs_, :], start=(nt == 0 and cs_ == 0),
                                                         stop=(nt == nt_ - 1 and cs_ == 1)),
                            reads=[UC, t], writes=[pY[pp * 2 + c]])
            for c in range(2):
                sop(kb, "act", lambda e: e.activation(out=ob[c][:], in_=pY[pp * 2 + c][:], func=AF.Copy, scale=scale),
                    reads=[pY[pp * 2 + c]], writes=[ob[c]])
                sstore(kb, "act", ob[c], g.oT[256 + c * 128:256 + (c + 1) * 128, kb_ * 512:(kb_ + 1) * 512], ob[c][:])


import os
GLA_CUT = float(os.environ.get('GLA_CUT', '99'))


def phase_gla(kb, g, l, s):
    t0, S = g.seq_off[s], g.seq_len[s]
    n = S // 128
    sc = float(32 ** -0.5)
    with kb.phase("gl"):
        B = lambda nm, sh, dt, ps=False: mkbuf(kb, nm, sh, dt, ps)
        mats = B("mats", [128, 2, 3, 128], BF16)
        cm = B("cm", [128, 2, 128], F32)
        hm = B("hm", [128, 4], F32)
        bd = B("bd", [128, 256], F32)
        nsix = B("nsix", [128, 2], BF16)
        ones1 = B("ones1", [128, 128], BF16)
        wg = B("wg", [128, 2, 128], BF16)
        bg = B("bg", [128, 2, 128], BF16)
        gout = B("gout", [128, 256], F32)
        zt = [B("zt%d" % i, [128, 768], F32) for i in range(2)]
        zg = [B("zg%d" % i, [128, 16], F32) for i in range(2)]
        gof = [B("gof%d" % i, [128, 256], F32) for i in range(2)]
        bank1 = kb.ps("bank1", [128, 512], F32)
        bank3 = kb.ps("bank3", [128, 6, 128], BF16)
        bank5 = kb.ps("bank5", [128, 512], F32)
        bA = Buf(kb, bank1, "bA"); bC = Buf(kb, bank3, "bC"); bE = Buf(kb, bank5, "bE")
        pTg = View(bC, bank3[0:16, 5, :], "pTg")
        zgT = B("zgT", [128, 128], BF16)
        zgb = B("zgb", [128, 16], BF16)
        glh = B("glh", [128, 128], BF16)
        gll = B("gll", [128, 128], BF16)
        px = View(bA, bank1[:, 128:256], "px")
        e1 = B("e1", [128, 128], F32)
        gl = B("gl", [128, 128], F32)
        pX = B("pX", [128, 3, 128], F32, True)
        pdec = View(bA, bank1[:, 256:258], "pdec")
        E = B("E", [128, 3, 128], F32)
        E2 = B("E2", [128, 128], F32)
        dec = B("dec", [128, 1], F32)
        qk4 = B("qk4", [128, 4, 128], BF16)
        pT3 = View(bC, bank3[:, 0:3, :], "pT3")
        tT = B("tT", [128, 3, 128], BF16)
        qibd = B("qibd", [128, 4, 128], BF16)
        pA = B("pA", [128, 4, 128], F32, True)
        ATs = B("ATs", [128, 4, 128], BF16)
        vb = B("vb", [128, 256], BF16)
        pO = View(bE, bank5[:, 0:256], "pO")
        pO2 = View(bE, bank5[:, 256:512], "pO2")
        pU = B("pU", [128, 256], F32, True)
        o2 = B("o2", [128, 256], F32)
        oc = [B("oc%d" % i, [128, 256], F32) for i in range(2)]
        Um = B("Um", [128, 256], F32)
        St = B("St", [128, 256], F32)
        Sb = B("Sb", [128, 256], BF16)
        sq = B("sq", [128, 256], F32)
        ssum = B("ssum", [128, 4], F32)
        rstd = B("rstd", [128, 4], F32)
        on = B("on", [128, 256], F32)
        sig = B("sig", [128, 256], F32)
        res = B("res", [128, 256], BF16)
        pTo = View(bC, bank3[:, 3:5, :], "pTo")
        oTt = [B("oTt%d" % i, [128, 2, 128], BF16) for i in range(2)]
        sload(kb, "sp", mats, mats[:], g.gla_mats)
        sload(kb, "sp", cm, cm[:], g.gla_cm)
        sload(kb, "sp", hm, hm[:], g.gla_hm)
        sload(kb, "sp", bd, bd[:], g.gla_bd)
        sop(kb, "dve", lambda e: e.memset(wg[:], 0.0), writes=[wg])
        sop(kb, "dve", lambda e: e.memset(bg[:], 0.0), writes=[bg])
        sop(kb, "dve", lambda e: e.memset(zgT[:], 0.0), writes=[zgT])
        wgf = B("wgf", [16, 2, 128], F32)
        bgf = B("bgf", [1, 2, 128], F32)
        sload(kb, "sp", wgf, wgf[:, 0, :], g.gla_w_gf[l])
        sload(kb, "sp", wgf, wgf[:, 1, :], g.gla_w_gb[l], group=True)
        sload(kb, "sp", bgf, bgf[:, 0, :], g.gla_b_gf[l:l + 1, :])
        sload(kb, "sp", bgf, bgf[:, 1, :], g.gla_b_gb[l:l + 1, :], group=True)
        sop(kb, "dve", lambda e: e.tensor_copy(out=wg[0:16, :, :], in_=wgf[:]), reads=[wgf], writes=[wg])
        sop(kb, "dve", lambda e: e.tensor_copy(out=bg[0:1, :, :], in_=bgf[:]), reads=[bgf], writes=[bg])
        sload(kb, "sp", gout, gout[:], g.gla_g_out[l].partition_broadcast(128))
        sop(kb, "pool", lambda e: e.memset(nsix[:], -1.0 / 16), writes=[nsix])
        sop(kb, "pool", lambda e: e.memset(ones1[:], 0.0), writes=[ones1])
        sop(kb, "pool", lambda e: e.memset(ones1[0:1, :], 1.0), writes=[ones1])
        cnt = 0
        for d in range(2):
            sop(kb, "pool", lambda e: e.memset(St[:], 0.0), writes=[St])
            sop(kb, "pool", lambda e: e.memset(Sb[:], 0.0), writes=[Sb])
            order = range(n) if d == 0 else range(n - 1, -1, -1)
            if d == 1:
                kb.wait("sp", [t_ for b_ in oc for t_ in b_.r if t_.ctr is b_.st])
            for i in order:
                p = cnt % 2
                cnt += 1
                row = t0 + i * 128
                z = zt[p]
                sload(kb, "sp", z, z[:], g.zs[row:row + 128, C_GQ:C_GQ + 768])
                zcol = C_ZF if d == 0 else C_ZB
                sload(kb, "sp", zg[p], zg[p][:], g.zs[row:row + 128, zcol:zcol + 16])
                if d == 1:
                    sload(kb, "sp", gof[p], gof[p][:], g.go[i * 128:(i + 1) * 128, :])
                sop(kb, "dve", lambda e: e.tensor_copy(out=zgb[:], in_=zg[p][:]), reads=[zg[p]], writes=[zgb])
                sop(kb, "pe", lambda e: e.transpose(out=pTg[:], in_=zgb[:], identity=g.identb[:]),
                    reads=[zgb], writes=[pTg])
                sop(kb, "act", lambda e: e.copy(out=zgT[0:16, :], in_=pTg[:]), reads=[pTg], writes=[zgT])
                if GLA_CUT <= 2:
                    continue
                sop(kb, "pe", lambda e: e.matmul(px[:], lhsT=ones1[:], rhs=bg[:, d, :], start=True, stop=False),
                    reads=[ones1, bg], writes=[px])
                sop(kb, "pe", lambda e: e.matmul(px[:], lhsT=zgT[:], rhs=wg[:, d, :], start=False, stop=True),
                    reads=[zgT, wg], writes=[px])
                if GLA_CUT <= 2.5:
                    continue
                sop(kb, "act", lambda e: e.activation(out=e1[:], in_=px[:], func=AF.Exp, scale=-1.0),
                    reads=[px], writes=[e1])
                if GLA_CUT <= 2.7:
                    continue
                sop(kb, "act", lambda e: e.activation(out=gl[:], in_=e1[:], func=AF.Ln, bias=1.0),
                    reads=[e1], writes=[gl])
                if GLA_CUT <= 3:
                    continue
                sop(kb, "dve", lambda e: e.tensor_copy(out=glh[:], in_=gl[:]), reads=[gl], writes=[glh])
                sop(kb, "dve", lambda e: e.tensor_tensor(out=gll[:], in0=gl[:], in1=glh[:], op=ALU.subtract),
                    reads=[gl, glh], writes=[gll])
                for j in range(3):
                    sop(kb, "pe", lambda e: e.matmul(pX[:, j, :], lhsT=mats[:, d, j, :], rhs=glh[:], start=True, stop=False),
                        reads=[mats, glh], writes=[pX])
                    sop(kb, "pe", lambda e: e.matmul(pX[:, j, :], lhsT=mats[:, d, j, :], rhs=gll[:], start=False, stop=True),
                        reads=[mats, gll], writes=[pX])
                sop(kb, "pe", lambda e: e.matmul(pdec[:], lhsT=glh[:], rhs=nsix[:], start=True, stop=False),
                    reads=[glh, nsix], writes=[pdec])
                sop(kb, "pe", lambda e: e.matmul(pdec[:], lhsT=gll[:], rhs=nsix[:], start=False, stop=True),
                    reads=[gll, nsix], writes=[pdec])
                if GLA_CUT <= 4:
                    continue
                sop(kb, "act", lambda e: e.activation(out=E[:], in_=pX[:], func=AF.Exp), reads=[pX], writes=[E])
                sop(kb, "act", lambda e: e.activation(out=E2[:], in_=pX[:, 0, :], func=AF.Exp, scale=-1.0),
                    reads=[pX], writes=[E2])
                sop(kb, "act", lambda e: e.activation(out=dec[:], in_=pdec[:, 0:1], func=AF.Exp), reads=[pdec], writes=[dec])
                if GLA_CUT <= 5:
                    continue
                stt = lambda o_, i0, i1: (lambda e: e.scalar_tensor_tensor(out=o_, in0=i0, scalar=sc, in1=i1,
                                                                          op0=ALU.mult, op1=ALU.mult))
                sop(kb, "dve", stt(qk4[:, 0, :], z[:, 0:128], E[:, 0, :]), reads=[z, E], writes=[qk4])
                sop(kb, "pool", lambda e: e.tensor_tensor(out=qk4[:, 1, :], in0=z[:, 128:256], in1=E2[:], op=ALU.mult),
                    reads=[z, E2], writes=[qk4])
                sop(kb, "dve", stt(qk4[:, 2, :], z[:, 0:128], E[:, 2, :]), reads=[z, E], writes=[qk4])
                sop(kb, "pool", lambda e: e.tensor_tensor(out=qk4[:, 3, :], in0=z[:, 128:256], in1=E[:, 1, :], op=ALU.mult),
                    reads=[z, E], writes=[qk4])
                sop(kb, "pool", lambda e: e.tensor_copy(out=vb[:], in_=z[:, 256:512]), reads=[z], writes=[vb])
                if GLA_CUT <= 6:
                    continue
                for j in range(3):
                    sop(kb, "pe", lambda e: e.transpose(out=pT3[:, j, :], in_=qk4[:, j, :], identity=g.identb[:]),
                        reads=[qk4], writes=[pT3])
                sop(kb, "act", lambda e: e.copy(out=tT[:], in_=pT3[:]), reads=[pT3], writes=[tT])
                for h in range(4):
                    sop(kb, "dve", lambda e: e.tensor_scalar(out=qibd[:, h, :], in0=tT[:, 0, :], scalar1=hm[:, h:h + 1], scalar2=None,
                                                             op0=ALU.mult), reads=[tT, hm], writes=[qibd])
                if GLA_CUT <= 7:
                    continue
                sop(kb, "pe", lambda e: e.matmul(pA[:].rearrange("p h t -> p (h t)"), lhsT=tT[:, 1, :],
                                                 rhs=qibd[:].rearrange("p h t -> p (h t)"), start=True, stop=True),
                    reads=[tT, qibd], writes=[pA])
                for h in range(4):
                    sop(kb, "dve", lambda e: e.tensor_tensor(out=ATs[:, h, :], in0=pA[:, h, :], in1=cm[:, d, :], op=ALU.mult),
                        reads=[pA, cm], writes=[ATs])
                if GLA_CUT <= 8:
                    continue
                for h in range(4):
                    sop(kb, "pe", lambda e: e.matmul(pO[:, h * 64:(h + 1) * 64], lhsT=ATs[:, h, :], rhs=vb[:, h * 64:(h + 1) * 64],
                                                     start=True, stop=True), reads=[ATs, vb], writes=[pO])
                sop(kb, "pe", lambda e: e.matmul(pO2[:], lhsT=tT[:, 2, :], rhs=Sb[:], start=True, stop=True),
                    reads=[tT, Sb], writes=[pO2])
                sop(kb, "pe", lambda e: e.matmul(pU[:], lhsT=qk4[:, 3, :], rhs=vb[:], start=True, stop=True),
                    reads=[qk4, vb], writes=[pU])
                if GLA_CUT <= 9:
                    continue
                sop(kb, "act", lambda e: e.copy(out=o2[:], in_=pO2[:]), reads=[pO2], writes=[o2])
                sop(kb, "dve", lambda e: e.tensor_tensor(out=oc[p][:], in0=pO[:], in1=o2[:], op=ALU.add),
                    reads=[pO, o2], writes=[oc[p]])
                sop(kb, "dve", lambda e: e.tensor_tensor(out=Um[:], in0=pU[:], in1=bd[:], op=ALU.mult),
                    reads=[pU, bd], writes=[Um])
                sop(kb, "dve", lambda e: e.scalar_tensor_tensor(out=St[:], in0=St[:], scalar=dec[:], in1=Um[:],
                                                                op0=ALU.mult, op1=ALU.add), reads=[St, dec, Um], writes=[St])
                sop(kb, "pool", lambda e: e.tensor_copy(out=Sb[:], in_=St[:]), reads=[St], writes=[Sb])
                if GLA_CUT <= 10:
                    continue
                if d == 0:
                    sstore(kb, "act", oc[p], g.go[i * 128:(i + 1) * 128, :], oc[p][:])
                    continue
                o = oc[p]
                sop(kb, "dve", lambda e: e.tensor_tensor(out=o[:], in0=o[:], in1=gof[p][:], op=ALU.add),
                    reads=[gof[p]], writes=[o])
                sop(kb, "pool", lambda e: e.tensor_tensor(out=sq[:], in0=o[:], in1=o[:], op=ALU.mult), reads=[o], writes=[sq])
                sop(kb, "dve", lambda e: e.tensor_reduce(out=ssum[:], in_=sq[:].rearrange("p (h c) -> p h c", h=4),
                                                         axis=AX.X, op=ALU.add), reads=[sq], writes=[ssum])
                sop(kb, "dve", lambda e: e.tensor_scalar(out=rstd[:], in0=ssum[:], scalar1=1.0 / 64, scalar2=EPS,
                                                         op0=ALU.mult, op1=ALU.add), reads=[ssum], writes=[rstd])
                sop(kb, "act", lambda e: e.activation(out=rstd[:], in_=rstd[:], func=AF.Ln), reads=[rstd], writes=[rstd])
                sop(kb, "act", lambda e: e.activation(out=rstd[:], in_=rstd[:], func=AF.Exp, scale=-0.5), reads=[rstd], writes=[rstd])
                v4 = lambda b_: b_[:].rearrange("p (h c) -> p h c", h=4)
                for h in range(4):
                    sop(kb, "dve", lambda e: e.tensor_scalar(out=on[:, h * 64:(h + 1) * 64], in0=o[:, h * 64:(h + 1) * 64],
                                                             scalar1=rstd[:, h:h + 1], scalar2=None, op0=ALU.mult),
                        reads=[o, rstd], writes=[on])
                sop(kb, "pool", lambda e: e.tensor_tensor(out=on[:], in0=on[:], in1=gout[:], op=ALU.mult),
                    reads=[gout], writes=[on])
                sop(kb, "act", lambda e: e.activation(out=sig[:], in_=z[:, 512:768], func=AF.Sigmoid), reads=[z], writes=[sig])
                sop(kb, "pool", lambda e: e.tensor_tensor(out=sig[:], in0=sig[:], in1=z[:, 512:768], op=ALU.mult),
                    reads=[z], writes=[sig])
                sop(kb, "dve", lambda e: e.tensor_tensor(out=res[:], in0=on[:], in1=sig[:], op=ALU.mult),
                    reads=[on, sig], writes=[res])
                transposes(kb, g, res, 2, pTo, oTt[p])
                sstore(kb, "act", oTt[p], g.oT[512:768, i * 128:(i + 1) * 128].rearrange("(c p) t -> p c t", p=128), oTt[p][:])


def wload_bf16(kb, buf, out_ap, in_ap, group=False):
    return sload(kb, "pool", buf, out_ap, in_ap, group=group)


PC_CUT = float(os.environ.get('PC_CUT', '99'))


def phase_c(kb, g, l, s):
    t0, S = g.seq_off[s], g.seq_len[s]
    src = g.x_in if l == 0 else g.xres
    with kb.phase("pc"):
        B = lambda nm, sh, dt, ps=False: mkbuf(kb, nm, sh, dt, ps)
        wout = B("wout", [128, 8, 1024], BF16)
        wr = B("wr", [128, 8, 32], BF16)
        br = B("br", [128, 32], BF16)
        ones1 = B("ones1", [128, 128], BF16)
        oTt = [B("oTt%d" % i, [128, 8, 128], BF16) for i in range(2)]
        xt = [B("xt%d" % i, [128, D], F32) for i in range(2)]
        py = B("py", [128, 2, 512], F32, True)
        tmp = B("tmp", [128, D], F32)
        x2 = [B("x2%d" % i, [128, D], F32) for i in range(2)]
        junk = B("junk", [128, D], BF16)
        ss = B("ss", [128, 1], F32); rs = B("rs", [128, 1], F32)
        hb = B("hb", [128, D], BF16)
        pT = B("pT", [128, 8, 128], BF16, True)
        hT = [B("hT%d" % i, [128, 8, 128], BF16) for i in range(2)]
        pl = B("pl", [128, 32], F32, True)
        lg = B("lg", [128, 32], F32)
        m8 = B("m8", [128, 8], F32)
        negm = B("negm", [128, 1], F32)
        mask = B("mask", [128, 32], F32)
        ex = B("ex", [128, 32], F32)
        sm = B("sm", [128, 1], F32)
        gt = [B("gt%d" % i, [128, 32], F32) for i in range(2)]
        woutf = B("woutf", [128, 8, 1024], F32)
        sload(kb, "sp", woutf, woutf[:], g.w_out[l].rearrange("(k p) n -> p k n", p=128))
        sop(kb, "dve", lambda e: e.tensor_copy(out=wout[:], in_=woutf[:]), reads=[woutf], writes=[wout])
        wrf = B("wrf", [128, 8, 32], F32)
        sload(kb, "sp", wrf, wrf[:], g.w_router[l].rearrange("(k p) n -> p k n", p=128))
        sop(kb, "dve", lambda e: e.tensor_copy(out=wr[:], in_=wrf[:]), reads=[wrf], writes=[wr])
        brf = B("brf", [1, 32], F32)
        sop(kb, "dve", lambda e: e.memset(br[:], 0.0), writes=[br])
        sload(kb, "sp", brf, brf[:], g.b_router[l:l + 1, :])
        sop(kb, "dve", lambda e: e.tensor_copy(out=br[0:1, :], in_=brf[:]), reads=[brf], writes=[br])
        sop(kb, "dve", lambda e: e.memset(ones1[:], 0.0), writes=[ones1])
        sop(kb, "dve", lambda e: e.memset(ones1[0:1, :], 1.0), writes=[ones1])
        mb = g.modbc_buf
        for i in range(S // 128):
            p = i % 2
            row = t0 + i * 128
            sload(kb, "sp", oTt[p], oTt[p][:], g.oT[:, i * 128:(i + 1) * 128].rearrange("(c p) t -> p c t", p=128))
            sload(kb, "sp", xt[p], xt[p][:], src[row:row + 128, :])
            for nb in range(2):
                for c in range(8):
                    sop(kb, "pe", lambda e: e.matmul(py[:, nb, :], lhsT=oTt[p][:, c, :], rhs=wout[:, c, nb * 512:(nb + 1) * 512],
                                                     start=(c == 0), stop=(c == 7)), reads=[oTt[p], wout], writes=[py])
            sop(kb, "dve", lambda e: e.tensor_tensor(out=tmp[:], in0=py[:].rearrange("p a b -> p (a b)"), in1=g.modbc[:, 2, :],
                                                     op=ALU.mult), reads=[py, mb], writes=[tmp])
            x = x2[p]
            sop(kb, "pool", lambda e: e.tensor_tensor(out=x[:], in0=tmp[:], in1=xt[p][:], op=ALU.add),
                reads=[tmp, xt[p]], writes=[x])
            sstore(kb, "act", x, g.xres[row:row + 128, :], x[:])
            if PC_CUT <= 1:
                continue
            sop(kb, "act", lambda e: e.activation(out=junk[:], in_=x[:], func=AF.Square, accum_out=ss[:]),
                reads=[x], writes=[junk, ss])
            sop(kb, "dve", lambda e: e.tensor_scalar(out=rs[:], in0=ss[:], scalar1=1.0 / D, scalar2=EPS,
                                                     op0=ALU.mult, op1=ALU.add), reads=[ss], writes=[rs])
            sop(kb, "act", lambda e: e.activation(out=rs[:], in_=rs[:], func=AF.Ln), reads=[rs], writes=[rs])
            sop(kb, "act", lambda e: e.activation(out=rs[:], in_=rs[:], func=AF.Exp, scale=-0.5), reads=[rs], writes=[rs])
            sop(kb, "dve", lambda e: e.scalar_tensor_tensor(out=tmp[:], in0=x[:], scalar=rs[:], in1=g.modbc[:, 4, :],
                                                            op0=ALU.mult, op1=ALU.mult), reads=[x, rs, mb], writes=[tmp])
            sop(kb, "pool", lambda e: e.tensor_tensor(out=hb[:], in0=tmp[:], in1=g.modbc[:, 3, :], op=ALU.add),
                reads=[tmp, mb], writes=[hb])
            transposes(kb, g, hb, 8, pT, hT[p])
            sstore(kb, "act", hT[p], g.h2T[:, :, row:row + 128], hT[p][:])
            if PC_CUT <= 2:
                continue
            sop(kb, "pe", lambda e: e.matmul(pl[:], lhsT=ones1[:], rhs=br[:], start=True, stop=False),
                reads=[ones1, br], writes=[pl])
            for c in range(8):
                sop(kb, "pe", lambda e: e.matmul(pl[:], lhsT=hT[p][:, c, :], rhs=wr[:, c, :], start=False, stop=(c == 7)),
                    reads=[hT[p], wr], writes=[pl])
            sop(kb, "act", lambda e: e.copy(out=lg[:], in_=pl[:]), reads=[pl], writes=[lg])
            if PC_CUT <= 3:
                continue
            sop(kb, "dve", lambda e: e.max(out=m8[:], in_=lg[:]), reads=[lg], writes=[m8])
            sop(kb, "dve", lambda e: e.tensor_scalar(out=mask[:], in0=lg[:], scalar1=m8[:, 3:4], scalar2=None, op0=ALU.is_ge),
                reads=[lg, m8], writes=[mask])
            sop(kb, "pool", lambda e: e.tensor_single_scalar(out=negm[:], in_=m8[:, 0:1], scalar=-1.0, op=ALU.mult),
                reads=[m8], writes=[negm])
            if PC_CUT <= 4:
                continue
            sop(kb, "act", lambda e: e.activation(out=ex[:], in_=lg[:], func=AF.Exp, bias=negm[:]),
                reads=[lg, negm], writes=[ex])
            sop(kb, "dve", lambda e: e.tensor_tensor(out=ex[:], in0=ex[:], in1=mask[:], op=ALU.mult),
                reads=[mask], writes=[ex])
            sop(kb, "dve", lambda e: e.reduce_sum(out=sm[:], in_=ex[:], axis=AX.X), reads=[ex], writes=[sm])
            sop(kb, "dve", lambda e: e.reciprocal(out=sm[:], in_=sm[:]), reads=[], writes=[sm])
            sop(kb, "dve", lambda e: e.tensor_scalar(out=gt[p][:], in0=ex[:], scalar1=sm[:], scalar2=None, op0=ALU.mult),
                reads=[ex, sm], writes=[gt[p]])
            sstore(kb, "act", gt[p], g.gates[row:row + 128, :], gt[p][:])


def ensure_dma(kb, buf, ld=True, st=False, sw=False):
    if ld and buf.ld is None:
        buf.ld = _static_base(kb, buf.name + "_ld", sw=sw)
    if st and buf.st is None:
        buf.st = _static_base(kb, buf.name + "_st")


def phase_moe(kb, g, l):
    NE = g.n_exp
    NSB = g.T // 1024
    WGU = 1024 * 2048
    WD = 1024 * 1024
    with kb.phase("mo"):
        B = lambda nm, sh, dt, ps=False: mkbuf(kb, nm, sh, dt, ps)
        hx = B("hx", [128, 8, 1024], BF16)
        acc = B("acc", [128, 8, 1024], F32)
        wgu = B("wgu", [128, 8, 2048], BF16)
        wd = B("wd", [128, 8, 1024], BF16)
        bgu = B("bgu", [128, 16], F32)
        bdn = B("bdn", [128, 1024], BF16)
        bdf = B("bdf", [1, 1024], F32)
        gcol = B("gcol", [128, 8], F32)
        ones1 = B("ones1", [128, 128], BF16)
        actT = B("actT", [128, 8, 1024], BF16)
        pg = [B("pg%d" % i, [128, 512], F32, True) for i in range(2)]
        pl_ = [B("pl%d" % i, [128, 512], F32, True) for i in range(2)]
        py = [B("py%d" % i, [128, 512], F32, True) for i in range(2)]
        gc = [B("gc%d" % i, [128, 512], F32) for i in range(2)]
        sg = [B("sg%d" % i, [128, 512], F32) for i in range(2)]
        lc = [B("lc%d" % i, [128, 512], F32) for i in range(2)]
        ty = B("ty", [128, 512], F32)
        stg = [B("stg%d" % i, [128, 8, 512], F32) for i in range(2)]
        xall = View(stg[0], stg[0].t[:].rearrange("p (a b) c -> p a (b c)", a=4), "xall")
        gtf = B("gtf", [128, D], F32)
        allb = [hx, acc, wgu, wd, bgu, bdn, bdf, gcol, ones1, actT, ty, gtf] + stg + pg + pl_ + py + gc + sg + lc
        for b_ in stg:
            ensure_dma(kb, b_, ld=True, st=True)
        for b_ in (bgu, gcol, hx, gtf, bdf):
            ensure_dma(kb, b_)

        def clear_tokens():
            for b_ in allb:
                b_.w, b_.r = [], []

        def body():
            for j in range(2):
                sload(kb, "act", stg[j], stg[j][:],
                      kb.dyn("act", 2, g.w_down[l, 0, :, j * 512:(j + 1) * 512].rearrange("(k p) n -> p k n", p=128), [(1, WD)]))
                sop(kb, "pool", lambda e: e.tensor_copy(out=wd[:, :, j * 512:(j + 1) * 512], in_=stg[j][:]),
                    reads=[stg[j]], writes=[wd])
            sload(kb, "act", bdf, bdf[:], kb.dyn("act", 1, g.b_down[l, 0:1, :], [(1, 1024)]))
            sop(kb, "dve", lambda e: e.tensor_copy(out=bdn[0:1, :], in_=bdf[:]), reads=[bdf], writes=[bdn])
            sload(kb, "sp", bgu, bgu[:], kb.dyn("sp", 0, g.b_gu_l[l, 0], [(1, 128 * 16)]))
            sload(kb, "sp", gcol, gcol[:],
                  kb.dyn("sp", 1, g.gates[0:1024, 0:1].rearrange("(t p) o -> p (t o)", p=128), [(0, 1024 * 32), (1, 1)]),
                  allow_slow_non_contiguous=True)
            cnt = 0
            for b in range(2):
                for c in range(8):
                    q = cnt % 2
                    cnt += 1
                    for k in range(8):
                        sop(kb, "pe", lambda e: e.matmul(pg[q][:], lhsT=wgu[:, k, c * 128:(c + 1) * 128],
                                                         rhs=hx[:, k, b * 512:(b + 1) * 512], start=(k == 0), stop=(k == 7)),
                            reads=[wgu, hx], writes=[pg[q]])
                    for k in range(8):
                        sop(kb, "pe", lambda e: e.matmul(pl_[q][:], lhsT=wgu[:, k, 1024 + c * 128:1024 + (c + 1) * 128],
                                                         rhs=hx[:, k, b * 512:(b + 1) * 512], start=(k == 0), stop=(k == 7)),
                            reads=[wgu, hx], writes=[pl_[q]])
                    sop(kb, "dve", lambda e: e.tensor_scalar(out=gc[q][:], in0=pg[q][:], scalar1=bgu[:, c:c + 1], scalar2=7.0,
                                                             op0=ALU.add, op1=ALU.min), reads=[pg[q], bgu], writes=[gc[q]])
                    sop(kb, "act", lambda e: e.activation(out=sg[q][:], in_=gc[q][:], func=AF.Sigmoid, scale=1.702),
                        reads=[gc[q]], writes=[sg[q]])
                    sop(kb, "dve", lambda e: e.tensor_scalar(out=lc[q][:], in0=pl_[q][:], scalar1=bgu[:, 8 + c:9 + c], scalar2=7.0,
                                                             op0=ALU.add, op1=ALU.min), reads=[pl_[q], bgu], writes=[lc[q]])
                    sop(kb, "pool", lambda e: e.tensor_scalar(out=lc[q][:], in0=lc[q][:], scalar1=-7.0, scalar2=1.0,
                                                              op0=ALU.max, op1=ALU.add), reads=[], writes=[lc[q]])
                    sop(kb, "pool", lambda e: e.tensor_tensor(out=sg[q][:], in0=sg[q][:], in1=gc[q][:], op=ALU.mult),
                        reads=[gc[q]], writes=[sg[q]])
                    sop(kb, "dve", lambda e: e.tensor_tensor(out=actT[:, c, b * 512:(b + 1) * 512], in0=sg[q][:], in1=lc[q][:],
                                                             op=ALU.mult), reads=[sg[q], lc[q]], writes=[actT])
            for j in range(4):
                sload(kb, "act", stg[j % 2], stg[j % 2][:],
                      kb.dyn("act", 2, g.w_gu[l, 0, :, j * 512:(j + 1) * 512].rearrange("(k p) n -> p k n", p=128), [],
                             pre=(1, 1, NE, WGU)))
                sop(kb, "pool", lambda e: e.tensor_copy(out=wgu[:, :, j * 512:(j + 1) * 512], in_=stg[j % 2][:]),
                    reads=[stg[j % 2]], writes=[wgu])
            cnt = 0
            for tt in range(8):
                for n_ in range(2):
                    q = cnt % 2
                    cnt += 1
                    sop(kb, "pe", lambda e: e.matmul(py[q][:], lhsT=ones1[:], rhs=bdn[:, n_ * 512:(n_ + 1) * 512],
                                                     start=True, stop=False), reads=[ones1, bdn], writes=[py[q]])
                    for c in range(8):
                        sop(kb, "pe", lambda e: e.matmul(py[q][:], lhsT=actT[:, c, tt * 128:(tt + 1) * 128],
                                                         rhs=wd[:, c, n_ * 512:(n_ + 1) * 512], start=False, stop=(c == 7)),
                            reads=[actT, wd], writes=[py[q]])
                    a_ = acc[:, tt, n_ * 512:(n_ + 1) * 512]
                    if q == 0:
                        sop(kb, "dve", lambda e: e.scalar_tensor_tensor(out=a_, in0=py[q][:], scalar=gcol[:, tt:tt + 1], in1=a_,
                                                                        op0=ALU.mult, op1=ALU.add),
                            reads=[py[q], gcol], writes=[acc])
                    else:
                        sop(kb, "act", lambda e: e.activation(out=ty[:], in_=py[q][:], func=AF.Copy, scale=gcol[:, tt:tt + 1]),
                            reads=[py[q], gcol], writes=[ty])
                        sop(kb, "pool", lambda e: e.tensor_tensor(out=a_, in0=a_, in1=ty[:], op=ALU.add),
                            reads=[ty], writes=[acc])

        for j in range(4):
            sload(kb, "act", stg[j % 2], stg[j % 2][:], g.w_gu[l, 0, :, j * 512:(j + 1) * 512].rearrange("(k p) n -> p k n", p=128))
            sop(kb, "pool", lambda e: e.tensor_copy(out=wgu[:, :, j * 512:(j + 1) * 512], in_=stg[j % 2][:]),
                reads=[stg[j % 2]], writes=[wgu])
        sop(kb, "dve", lambda e: e.memset(ones1[:], 0.0), writes=[ones1])
        sop(kb, "dve", lambda e: e.memset(ones1[0:1, :], 1.0), writes=[ones1])
        sop(kb, "dve", lambda e: e.memset(bdn[:], 0.0), writes=[bdn])
        kb.wait("pe", [ones1.w, bdn.w, wgu.w])
        keep = ()
        with kb.reset_loop(NSB, level=0, keep=keep):
            clear_tokens()
            sload(kb, "sp", hx, hx[:], kb.dyn("sp", 2, g.h2T[:, :, 0:1024], [(0, 1024)]))
            sload(kb, "sp", gtf, gtf[:], kb.dyn("sp", 0, g.gtf[0].partition_broadcast(128), [(0, 1024)]))
            sop(kb, "pool", lambda e: e.memset(acc[:], 0.0), writes=[acc])
            kb.wait("pe", [hx.w, acc.w, gtf.w])
            with kb.reset_loop(NE, level=1, keep=keep):
                clear_tokens()
                body()
            clear_tokens()
            for hf in range(2):
                xv = g.xres[hf * 512:(hf + 1) * 512, :].rearrange("(t p) d -> p t d", p=128)
                sload(kb, "sp", xall, xall[:], kb.dyn("sp", 1, xv, [(0, 1024 * D)]))
                for t4 in range(4):
                    tt = hf * 4 + t4
                    sop(kb, "dve", lambda e: e.tensor_tensor(out=acc[:, tt, :], in0=acc[:, tt, :], in1=gtf[:], op=ALU.mult),
                        reads=[gtf], writes=[acc])
                    sop(kb, "pool", lambda e: e.tensor_tensor(out=xall[:, t4, :], in0=xall[:, t4, :], in1=acc[:, tt, :], op=ALU.add),
                        reads=[acc], writes=[xall])
                sstore(kb, "sp", xall, kb.dyn("sp", 2, xv, [(0, 1024 * D)]), xall[:])
        clear_tokens()


def phase_final(kb, g):
    with kb.phase("fin"):
        B = lambda nm, sh, dt, ps=False: mkbuf(kb, nm, sh, dt, ps)
        gfin = B("gfin", [128, D], F32)
        xt = [B("xt%d" % i, [128, D], F32) for i in range(3)]
        junk = B("junk", [128, D], BF16)
        ss = [B("ss%d" % i, [128, 1], F32) for i in range(2)]
        rs = [B("rs%d" % i, [128, 1], F32) for i in range(2)]
        sload(kb, "sp", gfin, gfin[:], g.g_final.partition_broadcast(128))
        for i in range(g.T // 128):
            p = i % 3
            q = i % 2
            x = xt[p]
            sload(kb, "sp", x, x[:], g.xres[i * 128:(i + 1) * 128, :])
            sop(kb, "act", lambda e: e.activation(out=junk[:], in_=x[:], func=AF.Square, accum_out=ss[q][:]),
                reads=[x], writes=[junk, ss[q]])
            sop(kb, "dve", lambda e: e.tensor_scalar(out=rs[q][:], in0=ss[q][:], scalar1=1.0 / D, scalar2=EPS,
                                                     op0=ALU.mult, op1=ALU.add), reads=[ss[q]], writes=[rs[q]])
            sop(kb, "act", lambda e: e.activation(out=rs[q][:], in_=rs[q][:], func=AF.Ln), reads=[rs[q]], writes=[rs[q]])
            sop(kb, "act", lambda e: e.activation(out=rs[q][:], in_=rs[q][:], func=AF.Exp, scale=-0.5), reads=[rs[q]], writes=[rs[q]])
            sop(kb, "dve", lambda e: e.scalar_tensor_tensor(out=x[:], in0=x[:], scalar=rs[q][:], in1=gfin[:],
                                                            op0=ALU.mult, op1=ALU.mult), reads=[rs[q], gfin], writes=[x])
            sstore(kb, "act", x, g.y_out[i * 128:(i + 1) * 128, :], x[:])


SEQ_LENS = [4096, 4096, 8192]
N_CORES = 8
_CACHE = {}


def kernel(x_prompt, x_sample, c_prompt, c_sample, w_ada, b_ada, g_mix, g_ffn, w_in,
           mla_g_q, mla_g_kv, mla_w_uq, mla_w_ukv, gla_w_gf, gla_b_gf, gla_w_gb, gla_b_gb, gla_g_out,
           w_out, w_router, b_router, w_gu, b_gu, w_down, b_down, g_final):
    f32 = lambda a: np.ascontiguousarray(np.asarray(a, dtype=np.float32))
    x_prompt, x_sample, c_prompt, c_sample = f32(x_prompt), f32(x_sample), f32(c_prompt), f32(c_sample)
    weights = dict(w_ada=w_ada, b_ada=b_ada, g_mix=g_mix, g_ffn=g_ffn, w_in=w_in, mla_g_q=mla_g_q, mla_g_kv=mla_g_kv,
                   mla_w_uq=mla_w_uq, mla_w_ukv=mla_w_ukv, gla_w_gf=gla_w_gf, gla_b_gf=gla_b_gf, gla_w_gb=gla_w_gb,
                   gla_b_gb=gla_b_gb, gla_g_out=gla_g_out, w_out=w_out, w_router=w_router, b_router=b_router,
                   w_gu=w_gu, b_gu=b_gu, w_down=w_down, b_down=b_down, g_final=g_final)
    weights = {k: f32(v) for k, v in weights.items()}
    if "nc" not in _CACHE:
        _CACHE["nc"] = build(SEQ_LENS, depth=2, n_exp=32)
        _CACHE["consts"] = make_consts(max(SEQ_LENS))
    nc = _CACHE["nc"]
    consts = _CACHE["consts"]
    b_gu_l = np.ascontiguousarray(weights["b_gu"].reshape(2, 32, 16, 128).transpose(0, 1, 3, 2))
    clay = lambda c: np.ascontiguousarray(c.reshape(8, 128).T)
    in_maps = []
    for core in range(N_CORES):
        si = core % 2
        xs = np.concatenate([x_prompt[2 * core], x_prompt[2 * core + 1], x_sample[si]], axis=0)
        cs = np.stack([clay(c_prompt[2 * core]), clay(c_prompt[2 * core + 1]), clay(c_sample[si])])
        m = {"x_in": xs, "c_in": cs, "b_gu_l": b_gu_l}
        m.update(weights)
        m.update(consts)
        in_maps.append(m)
    res = run_bass_kernel_spmd(nc, in_maps, core_ids=list(range(N_CORES)))
    ys = [np.asarray(r["y_out"]) for r in res.results]
    y_prompt = np.stack([ys[c][j * 4096:(j + 1) * 4096] for c in range(N_CORES) for j in range(2)]).astype(np.float32)
    y_sample = np.stack([ys[0][8192:16384], ys[1][8192:16384]]).astype(np.float32)
    return (y_prompt, y_sample)
```
